# Optimizing a Trainium2 kernel written in Bass

```python
import jax, jax.numpy as jnp
from jax import lax
import numpy as np


D_MODEL = 1024
BATCH = 2
SEQ = 8192
DEPTH = 1

ATTN_HEADS = 16
HEAD_DIM = 64
ATTN_WIDTH = ATTN_HEADS * HEAD_DIM
DILATED_PATTERNS = ((128, 1), (512, 4), (2048, 16))
SSD_HEADS = 16
SSD_HEAD_DIM = 64
SSD_WIDTH = SSD_HEADS * SSD_HEAD_DIM
SSD_GROUPS = 2
SSD_HEADS_PER_GROUP = SSD_HEADS // SSD_GROUPS
SSD_STATE = 128
SSD_CHUNK = 128
CONV_WIDTH = 5
XBC_WIDTH = SSD_WIDTH + 2 * SSD_GROUPS * SSD_STATE
MIX_WIDTH = ATTN_WIDTH + SSD_WIDTH
IN_PROJ_WIDTH = 3 * ATTN_WIDTH + SSD_WIDTH + XBC_WIDTH + 2 * SSD_HEADS
N_EXPERTS = 32
TOP_K = 4
EXPERT_FF = 1024
SWIGLU_ALPHA = 1.702
SWIGLU_LIMIT = 7.0
RMS_EPS = 1e-6
N_MOD = 6

kernel_name = 'hybrid_dilated_attn_ssd_moe_block'


def _rmsnorm(t, g):
    var = jnp.mean(t * t, axis=-1, keepdims=True)
    return t * lax.rsqrt(var + RMS_EPS) * g


def _alibi_slopes(n):
    return jnp.exp2(-8.0 * jnp.arange(1, n + 1, dtype=jnp.float32) / n)


def _dilated_window_attention(q, k, v, slopes, window, dilation):
    bsz, nh, seq, hd = q.shape
    half = window // (2 * dilation)
    blk = half
    sub_len = seq // dilation
    nb = -(-sub_len // blk)
    pad_len = nb * blk

    def to_sub(t):
        t = t.reshape(bsz, nh, sub_len, dilation, hd).transpose(0, 1, 3, 2, 4)
        return jnp.pad(t, ((0, 0), (0, 0), (0, 0), (0, pad_len - sub_len), (0, 0)))

    qs, ks, vs = to_sub(q), to_sub(k), to_sub(v)
    qb = qs.reshape(bsz, nh, dilation, nb, blk, hd)

    def band(t):
        tp = jnp.pad(t, ((0, 0), (0, 0), (0, 0), (blk, blk), (0, 0)))
        tp = tp.reshape(bsz, nh, dilation, nb + 2, blk, hd)
        return jnp.concatenate([tp[:, :, :, 0:nb], tp[:, :, :, 1:nb + 1], tp[:, :, :, 2:nb + 2]], axis=4)

    kb, vb = band(ks), band(vs)
    scores = jnp.einsum('bhrnqe,bhrnke->bhrnqk', qb, kb)
    q_idx = jnp.arange(nb)[:, None] * blk + jnp.arange(blk)[None, :]
    k_idx = jnp.arange(nb)[:, None] * blk - blk + jnp.arange(3 * blk)[None, :]
    rel = k_idx[:, None, :] - q_idx[:, :, None]
    valid = (jnp.abs(rel) <= half) & (k_idx >= 0)[:, None, :] & (k_idx < sub_len)[:, None, :]
    dist = (jnp.abs(rel) * dilation).astype(jnp.float32)
    scores = scores - slopes[None, :, None, None, None, None] * dist
    scores = jnp.where(valid, scores, -jnp.inf)
    m = jnp.max(scores, axis=-1, keepdims=True)
    p = jnp.exp(scores - m)
    den = jnp.sum(p, axis=-1, keepdims=True)
    o = jnp.einsum('bhrnqk,bhrnke->bhrnqe', p, vb) / den
    lse = (m + jnp.log(den))[..., 0]

    def from_sub(t):
        t = t.reshape(bsz, nh, dilation, pad_len, *t.shape[5:])[:, :, :, :sub_len]
        t = jnp.swapaxes(t, 2, 3)
        return t.reshape(bsz, nh, seq, *t.shape[4:])

    return from_sub(o), from_sub(lse)


def _dilated_mixture_attention(q, k, v):
    slopes = _alibi_slopes(ATTN_HEADS)
    outs, lses = [], []
    for window, dilation in DILATED_PATTERNS:
        o, l = _dilated_window_attention(q, k, v, slopes, window, dilation)
        outs.append(o)
        lses.append(l)
    w = jax.nn.softmax(jnp.stack(lses), axis=0)
    return jnp.einsum('gbhs,gbhse->bhse', w, jnp.stack(outs))


def _ssd_chunked(xs, dt, a, bm, cm):
    bsz, seq = xs.shape[0], xs.shape[1]
    qn = SSD_CHUNK
    nc = seq // qn
    g, e, pdim, n = SSD_GROUPS, SSD_HEADS_PER_GROUP, SSD_HEAD_DIM, SSD_STATE
    xd = (xs * dt[..., None]).reshape(bsz, nc, qn, g, e, pdim)
    a_dt = jnp.moveaxis((dt * a).reshape(bsz, nc, qn, g, e), 2, -1)
    bc = bm.reshape(bsz, nc, qn, g, n)
    cc = cm.reshape(bsz, nc, qn, g, n)
    a_cum = jnp.cumsum(a_dt, axis=-1)
    tril = jnp.tril(jnp.ones((qn, qn), dtype=bool))
    seg = a_cum[..., :, None] - a_cum[..., None, :]
    decay_in = jnp.exp(jnp.where(tril, seg, -jnp.inf))
    cb = jnp.einsum('bclgn,bcsgn->bcgls', cc, bc)
    y_diag = jnp.einsum('bcgels,bcsgep->bclgep', cb[:, :, :, None] * decay_in, xd)
    decay_states = jnp.exp(a_cum[..., -1:] - a_cum)
    xw = xd * jnp.moveaxis(decay_states, -1, 2)[..., None]
    states = jnp.einsum('bcsgn,bcsgep->bcgepn', bc, xw)
    chunk_decay = jnp.exp(a_cum[..., -1])

    def step(h, inp):
        st, dec = inp
        return h * dec[..., None, None] + st, h

    h0 = jnp.zeros((bsz, g, e, pdim, n), dtype=xs.dtype)
    _, h_prev = lax.scan(step, h0, (jnp.moveaxis(states, 1, 0), jnp.moveaxis(chunk_decay, 1, 0)))
    h_prev = jnp.moveaxis(h_prev, 0, 1)
    y_off = jnp.einsum('bclgn,bcgepn->bclgep', cc, h_prev) * jnp.moveaxis(jnp.exp(a_cum), -1, 2)[..., None]
    return (y_diag + y_off).reshape(bsz, seq, g, e, pdim)


def _token_mixer(h, w_in, conv_w, conv_b, dt_bias, a_log, d_skip, g_ssd_norm, w_out):
    bsz, seq, _ = h.shape
    proj = h @ w_in
    cuts = [ATTN_WIDTH, 2 * ATTN_WIDTH, 3 * ATTN_WIDTH, 3 * ATTN_WIDTH + SSD_WIDTH,
            3 * ATTN_WIDTH + SSD_WIDTH + XBC_WIDTH]
    q, k, v, z, xbc, dt_raw = jnp.split(proj, cuts, axis=-1)

    def heads(t):
        return t.reshape(bsz, seq, ATTN_HEADS, HEAD_DIM).transpose(0, 2, 1, 3)

    attn = _dilated_mixture_attention(heads(q) * HEAD_DIM ** -0.5, heads(k), heads(v))
    attn = attn.transpose(0, 2, 1, 3).reshape(bsz, seq, ATTN_WIDTH)

    pad = CONV_WIDTH // 2
    xbc_p = jnp.pad(xbc, ((0, 0), (pad, pad), (0, 0)))
    conv = conv_b
    for i in range(CONV_WIDTH):
        conv = conv + xbc_p[:, i:i + seq] * conv_w[i]
    xbc = jax.nn.silu(conv)
    gh = (SSD_GROUPS, SSD_HEADS_PER_GROUP)
    xs = xbc[..., :SSD_WIDTH].reshape(bsz, seq, SSD_GROUPS, SSD_HEADS_PER_GROUP, SSD_HEAD_DIM)
    bm = xbc[..., SSD_WIDTH:SSD_WIDTH + SSD_GROUPS * SSD_STATE].reshape(bsz, seq, SSD_GROUPS, SSD_STATE)
    cm = xbc[..., SSD_WIDTH + SSD_GROUPS * SSD_STATE:].reshape(bsz, seq, SSD_GROUPS, SSD_STATE)
    dt = jax.nn.softplus(dt_raw.reshape(bsz, seq, 2, SSD_HEADS) + dt_bias)
    a = -jnp.exp(a_log)

    def flip(t):
        return jnp.flip(t, axis=1)

    y_fwd = _ssd_chunked(xs, dt[:, :, 0].reshape(bsz, seq, *gh), a[0].reshape(gh), bm, cm)
    y_bwd = flip(_ssd_chunked(flip(xs), flip(dt[:, :, 1].reshape(bsz, seq, *gh)),
                              a[1].reshape(gh), flip(bm), flip(cm)))
    y = y_fwd + y_bwd + d_skip.reshape(gh)[:, :, None] * xs
    y = y.reshape(bsz, seq, SSD_WIDTH)
    y = _rmsnorm(y * jax.nn.silu(z), g_ssd_norm)

    return jnp.concatenate([attn, y], axis=-1) @ w_out


def _moe(h, w_router, b_router, w_gate_up, b_gate_up, w_down, b_down):
    bsz, seq, dm = h.shape
    xt = h.reshape(bsz * seq, dm)
    logits = xt @ w_router + b_router
    top_val, top_idx = lax.top_k(logits, TOP_K)
    top_w = jax.nn.softmax(top_val, axis=-1)
    combine = jnp.sum(jax.nn.one_hot(top_idx, N_EXPERTS, dtype=xt.dtype) * top_w[..., None], axis=1)
    out = jnp.zeros_like(xt)
    for e in range(N_EXPERTS):
        gu = xt @ w_gate_up[e] + b_gate_up[e]
        gate = jnp.minimum(gu[:, :EXPERT_FF], SWIGLU_LIMIT)
        up = jnp.clip(gu[:, EXPERT_FF:], -SWIGLU_LIMIT, SWIGLU_LIMIT)
        act = (up + 1.0) * gate * jax.nn.sigmoid(SWIGLU_ALPHA * gate)
        out = out + combine[:, e:e + 1] * (act @ w_down[e] + b_down[e])
    return out.reshape(bsz, seq, dm)


def setup_inputs(seed: int = 0) -> dict:
    key = jax.random.key(seed)
    ks = jax.random.split(key, 24)
    f32 = jnp.float32

    def nrm(k, shape, scale):
        return jax.random.normal(k, shape, f32) * scale

    L = DEPTH
    x = nrm(ks[0], (BATCH, SEQ, D_MODEL), 1.0)
    c = nrm(ks[1], (BATCH, D_MODEL), 1.0)
    w_ada = nrm(ks[2], (L, D_MODEL, N_MOD * D_MODEL), 0.5 * D_MODEL ** -0.5)
    b_ada = nrm(ks[3], (L, N_MOD * D_MODEL), 0.02)
    g_pre_mix = 1.0 + nrm(ks[4], (L, D_MODEL), 0.05)
    g_post_mix = 1.0 + nrm(ks[5], (L, D_MODEL), 0.05)
    w_in_main = nrm(ks[6], (L, D_MODEL, IN_PROJ_WIDTH - 2 * SSD_HEADS), D_MODEL ** -0.5)
    w_in_dt = nrm(ks[7], (L, D_MODEL, 2 * SSD_HEADS), 0.1 * D_MODEL ** -0.5)
    w_in = jnp.concatenate([w_in_main, w_in_dt], axis=-1)
    conv_w = nrm(ks[8], (L, CONV_WIDTH, XBC_WIDTH), CONV_WIDTH ** -0.5)
    conv_b = nrm(ks[9], (L, XBC_WIDTH), 0.02)
    dt0 = jnp.exp(jax.random.uniform(ks[10], (L, 2, SSD_HEADS), f32,
                                     minval=float(np.log(1e-3)), maxval=float(np.log(1e-1))))
    dt_bias = dt0 + jnp.log(-jnp.expm1(-dt0))
    a_log = jnp.log(jax.random.uniform(ks[11], (L, 2, SSD_HEADS), f32, minval=1.0, maxval=16.0))
    d_skip = 1.0 + nrm(ks[12], (L, SSD_HEADS), 0.1)
    g_ssd_norm = 1.0 + nrm(ks[13], (L, SSD_WIDTH), 0.05)
    w_out = nrm(ks[14], (L, MIX_WIDTH, D_MODEL), MIX_WIDTH ** -0.5)
    g_pre_ffn = 1.0 + nrm(ks[15], (L, D_MODEL), 0.05)
    g_post_ffn = 1.0 + nrm(ks[16], (L, D_MODEL), 0.05)
    w_router = nrm(ks[17], (L, D_MODEL, N_EXPERTS), D_MODEL ** -0.5)
    b_router = nrm(ks[18], (L, N_EXPERTS), 0.01)
    w_gate_up = nrm(ks[19], (L, N_EXPERTS, D_MODEL, 2 * EXPERT_FF), D_MODEL ** -0.5)
    b_gate_up = nrm(ks[20], (L, N_EXPERTS, 2 * EXPERT_FF), 0.02)
    w_down = nrm(ks[21], (L, N_EXPERTS, EXPERT_FF, D_MODEL), EXPERT_FF ** -0.5)
    b_down = nrm(ks[22], (L, N_EXPERTS, D_MODEL), 0.02)
    return {'x': x, 'c': c, 'w_ada': w_ada, 'b_ada': b_ada,
            'g_pre_mix': g_pre_mix, 'g_post_mix': g_post_mix, 'w_in': w_in,
            'conv_w': conv_w, 'conv_b': conv_b, 'dt_bias': dt_bias, 'a_log': a_log,
            'd_skip': d_skip, 'g_ssd_norm': g_ssd_norm, 'w_out': w_out,
            'g_pre_ffn': g_pre_ffn, 'g_post_ffn': g_post_ffn,
            'w_router': w_router, 'b_router': b_router, 'w_gate_up': w_gate_up,
            'b_gate_up': b_gate_up, 'w_down': w_down, 'b_down': b_down}


def reference(x, c, w_ada, b_ada, g_pre_mix, g_post_mix, w_in, conv_w, conv_b, dt_bias,
              a_log, d_skip, g_ssd_norm, w_out, g_pre_ffn, g_post_ffn, w_router, b_router,
              w_gate_up, b_gate_up, w_down, b_down):
    f32 = jnp.float32
    out_dtype = x.dtype
    res = x.astype(f32)
    cs = jax.nn.silu(c.astype(f32))
    for layer in range(DEPTH):
        def p(t):
            return t[layer].astype(f32)

        mod = cs @ p(w_ada) + p(b_ada)
        shift_m, scale_m, gate_m, shift_f, scale_f, gate_f = [t[:, None, :] for t in jnp.split(mod, N_MOD, axis=-1)]
        h = _rmsnorm(res, p(g_pre_mix)) * (1.0 + scale_m) + shift_m
        mix = _token_mixer(h, p(w_in), p(conv_w), p(conv_b), p(dt_bias), p(a_log),
                           p(d_skip), p(g_ssd_norm), p(w_out))
        res = res + gate_m * _rmsnorm(mix, p(g_post_mix))
        h = _rmsnorm(res, p(g_pre_ffn)) * (1.0 + scale_f) + shift_f
        ffn = _moe(h, p(w_router), p(b_router), p(w_gate_up), p(b_gate_up), p(w_down), p(b_down))
        res = res + gate_f * _rmsnorm(ffn, p(g_post_ffn))
    return res.astype(out_dtype)
```

```python
from contextlib import ExitStack
import numpy as np
import concourse.bass as bass
import concourse.mybir as mybir
from concourse.bass_utils import run_bass_kernel_spmd

F32 = mybir.dt.float32
BF16 = mybir.dt.bfloat16
AF = mybir.ActivationFunctionType
ALU = mybir.AluOpType
AX = mybir.AxisListType

NCORES = 8
TOK = 2048
NT = 16
NTH = 32
EPS = 1e-6
NE = 32


class Rec:
    def __init__(self, nc, es):
        self.nc = nc
        self.es = es
        self.q = {e: [] for e in ("pe", "act", "dve", "pool", "sp")}
        self.semh = {}
        self.cnt = {}
        self.waited = {e: {} for e in self.q}
        self.lw = {}
        self.rd = {}

    def sem(self, key):
        if key not in self.semh:
            self.semh[key] = self.es.enter_context(self.nc.semaphore("s_" + key))
            self.cnt[key] = 0
        return self.semh[key]

    def op(self, eng, fn, r=(), w=(), semkey=None, inc=1):
        semkey = semkey or eng
        self.sem(semkey)
        deps = []
        for k in r:
            if k in self.lw:
                deps.append(self.lw[k])
        for k in w:
            if k in self.lw:
                deps.append(self.lw[k])
            deps += self.rd.get(k, [])
        need = {}
        for (sk, v) in deps:
            if sk.startswith("d_"):
                v = max(v, self.cnt[sk])
            if v > self.waited[eng].get(sk, 0):
                need[sk] = max(need.get(sk, 0), v)
        for sk, v in need.items():
            self.waited[eng][sk] = v
        self.cnt[semkey] += (inc if inc else 1)
        tok = (semkey, self.cnt[semkey])
        self.q[eng].append((list(need.items()), fn, semkey, inc))
        for k in r:
            self.rd.setdefault(k, []).append(tok)
        for k in w:
            self.lw[k] = tok
            self.rd[k] = []
        return tok

    def dma(self, eng, fn, r=(), w=(), sem="d_x"):
        return self.op(eng, fn, r, w, semkey=sem, inc=16)

    def flush(self):
        nc = self.nc
        need = [(sk, v) for sk, v in self.cnt.items() if sk.startswith("d_") and v > self.waited["sp"].get(sk, 0)]
        if need:
            for sk, v in need:
                self.waited["sp"][sk] = v
            self.sem("sp")
            self.cnt["sp"] += 1
            self.q["sp"].append((need, lambda e: e.nop(), "sp", 1))
        q = self.q
        semh = self.semh

        def run(engobj, lst):
            for waits, fn, semkey, inc in lst:
                for sk, v in waits:
                    engobj.wait_ge(semh[sk], v)
                ins = fn(engobj)
                if inc is None:
                    ins.then_inc(semh[semkey])
                else:
                    ins.then_inc(semh[semkey], inc)

        with nc.Block() as block:
            @block.tensor
            def _(e):
                run(e, q["pe"])

            @block.scalar
            def _(e):
                run(e, q["act"])

            @block.vector
            def _(e):
                run(e, q["dve"])

            @block.gpsimd
            def _(e):
                run(e, q["pool"])

            @block.sync
            def _(e):
                run(e, q["sp"])

        self.q = {e: [] for e in q}


def build(dbg=(), stop_after=99):
    nc = bass.Bass("TRN2", target_bir_lowering=False)
    es = ExitStack()
    R = Rec(nc, es)
    dbg_outs = {}

    def din(name, shape, dt=F32):
        return nc.dram_tensor(name, list(shape), dt, kind="ExternalInput").ap()

    CAPA, CAPB = 16384, 35500
    arenaA = es.enter_context(nc.sbuf_tensor("arenaA", [128, CAPA], F32))
    arenaB = es.enter_context(nc.sbuf_tensor("arenaB", [128, CAPB], F32))
    top = {"A": 0, "B": 0}
    cap = {"A": CAPA, "B": CAPB}

    def sb(name, shape, dt=F32, ar="B"):
        shape = list(shape)
        n = int(np.prod(shape[1:]))
        w = n if dt == F32 else (n + 1) // 2
        w = (w + 7) // 8 * 8
        off = top[ar]
        top[ar] += w
        assert top[ar] <= cap[ar], (name, ar, top[ar])
        base = arenaA if ar == "A" else arenaB
        ap = base[0:shape[0], off:off + w]
        if dt != F32:
            ap = ap.bitcast(dt)
        ap = ap[:, 0:n]
        if len(shape) == 3:
            ap = ap.rearrange("p (a b) -> p a b", a=shape[1])
        elif len(shape) == 4:
            ap = ap.rearrange("p (a b c) -> p a b c", a=shape[1], b=shape[2])
        return ap

    class Mark:
        def __init__(self, ar="B"):
            self.ar = ar

        def __enter__(self):
            self.m = top[self.ar]
            return self

        def __exit__(self, *a):
            top[self.ar] = self.m
            return False

    def v3(ap):
        return ap.rearrange("p (h e) -> p h e", e=64)

    xh = din("xh", [4096, 1024])
    valid_d = din("valid", [128, 32])
    edge_d = din("edge", [128, 2])
    sel_d = din("sel", [128, 4])
    cvec_d = din("cvec", [128, 8])
    w_ada = din("w_ada", [1024, 6144]).rearrange("(k p) n -> p k n", p=128)
    b_ada_row = din("b_ada_row", [6144])
    gpre_mix_row = din("g_pre_mix", [1024])
    gpre_ffn_row = din("g_pre_ffn", [1024])
    gpost_mix_row = din("g_post_mix", [1024])
    gpost_ffn_row = din("g_post_ffn", [1024])
    gssd_row = din("g_ssd_norm", [1024])
    w_in = din("w_in", [1024, 5664]).rearrange("(k p) n -> p k n", p=128)
    convw_d = din("convw", [128, 60])
    convb_d = din("convb", [128, 12])
    dtb_row = din("dt_bias", [32])
    alog_row = din("a_log", [32])
    dskip_row = din("d_skip", [16])
    w_out = din("w_out", [2048, 1024]).rearrange("(k p) n -> p k n", p=128)
    w_router = din("w_router", [1024, 32]).rearrange("(k p) n -> p k n", p=128)
    b_router_row = din("b_router", [32])
    w_gu = din("w_gate_up", [32, 1024, 2048]) if stop_after >= 6 else None
    w_dn = din("w_down", [32, 1024, 1024]) if stop_after >= 6 else None
    bguF_d = din("bguF", [128, 512])
    b_down_d = din("b_down", [32, 1024])
    ident_d = din("ident", [128, 128])
    masks_d = din("masks", [128, 5 * 128])
    etab_d = din("etab", [16, 128, 17 * 128])
    out_d = nc.dram_tensor("out", [TOK, 1024], F32, kind="ExternalOutput").ap()
    res1_d = nc.dram_tensor("res1_scr", [TOK, 1024], F32).ap()
    attn_d = nc.dram_tensor("attn_scr", [TOK, 1024], BF16).ap()
    zs_d = nc.dram_tensor("zs_scr", [TOK, 1024], BF16).ap()
    comb_d = nc.dram_tensor("comb_scr", [32, TOK], F32).ap()
    bnc_in = nc.dram_tensor("bnc_in", [128, 2048], F32).ap()
    bnc_out = nc.dram_tensor("bnc_out", [512, 2048], F32).ap()
    bnc2_in = nc.dram_tensor("bnc2_in", [128, 64], F32).ap()
    bnc2_out = nc.dram_tensor("bnc2_out", [512, 64], F32).ap()

    def dump(name, ap_sb, shape, rkeys, dt=F32):
        if name not in dbg:
            return
        d = nc.dram_tensor("dbg_" + name, list(shape), dt, kind="ExternalOutput").ap()
        dbg_outs[name] = d
        R.dma("sp", lambda e: e.dma_start(out=d, in_=ap_sb), r=rkeys, w=("dbgout_" + name,), sem="d_dbg")

    def finish():
        if R.q["sp"] or R.q["pe"] or R.q["dve"] or R.q["act"] or R.q["pool"]:
            R.flush()
        return nc, es, dbg_outs

    ps = [es.enter_context(nc.psum_tensor(f"ps{i}", [128, 512], F32)) for i in range(8)]

    def gsb(name, shape, dt=F32):
        return es.enter_context(nc.sbuf_tensor("g_" + name, list(shape), dt))
    ident = gsb("ident", [128, 128])
    identb = gsb("identb", [128, 128], BF16)
    masks = gsb("masks", [128, 5, 128])
    triLE, triGE, mSU, mSL, ones = (masks[:, i, :] for i in range(5))
    valid = gsb("valid", [128, 32])
    edge = gsb("edge", [128, 8])
    sel = gsb("sel", [128, 8])
    Gf = sb("Gf", [128, 1024])
    Arow_f = sb("Arow_f", [128, 1024])
    Brow_f = sb("Brow_f", [128, 1024])
    Gm = sb("Gm", [128, 1024])
    gssd = sb("gssd", [128, 1024])
    baseB = top["B"]
    Arow_m = sb("Arow_m", [128, 1024])
    Brow_m = sb("Brow_m", [128, 1024])

    R.dma("sp", lambda e: e.dma_start(out=ident[:], in_=ident_d), w=("ident",), sem="d_c")
    R.dma("sp", lambda e: e.dma_start(out=masks[:], in_=masks_d.rearrange("p (a b) -> p a b", a=5)), w=("masks",), sem="d_c")
    R.dma("sp", lambda e: e.dma_start(out=valid[:], in_=valid_d), w=("valid",), sem="d_c")
    R.dma("sp", lambda e: e.dma_start(out=edge[:, 0:2], in_=edge_d), w=("edge",), sem="d_c")
    R.dma("sp", lambda e: e.dma_start(out=sel[:, 0:4], in_=sel_d), w=("sel",), sem="d_c")
    R.dma("sp", lambda e: e.dma_start(out=gssd, in_=gssd_row.partition_broadcast(128)), w=("gssd",), sem="d_c")
    R.op("dve", lambda e: e.tensor_copy(out=identb[:], in_=ident[:]), r=("ident",), w=("identb",))

    with Mark():
        cvec = sb("cvec", [128, 8])
        cs = sb("cs", [128, 8])
        csB = sb("csB", [128, 8, 128])
        wada = [sb(f"wada{i}", [128, 3072]) for i in range(2)]
        brow = sb("brow", [128, 3072])
        g1 = sb("g1", [128, 1024])
        g2 = sb("g2", [128, 1024])
        t1 = sb("t1m", [128, 1024])
        R.dma("sp", lambda e: e.dma_start(out=cvec, in_=cvec_d), w=("cvec",), sem="d_c")
        R.op("act", lambda e: e.activation(out=cs, in_=cvec, func=AF.Silu), r=("cvec",), w=("cs",))
        for k in range(8):
            R.op("dve", lambda e, k=k: e.tensor_copy(out=csB[:, k, :], in_=cs[:, k:k + 1].to_broadcast([128, 128])),
                 r=("cs",), w=(f"csB{k}",))
        for pas in range(2):
            c0 = pas * 3072
            Brow, Arow, Grow = (Brow_m, Arow_m, Gm) if pas == 0 else (Brow_f, Arow_f, Gf)
            R.dma("sp", lambda e, c0=c0: e.dma_start(out=brow, in_=b_ada_row[c0:c0 + 3072].partition_broadcast(128)), w=("brow",), sem="d_c")
            R.dma("sp", lambda e, pas=pas: e.dma_start(out=g1, in_=(gpre_mix_row if pas == 0 else gpre_ffn_row).partition_broadcast(128)),
                  w=("g1",), sem="d_c")
            R.dma("sp", lambda e, pas=pas: e.dma_start(out=g2, in_=(gpost_mix_row if pas == 0 else gpost_ffn_row).partition_broadcast(128)),
                  w=("g2",), sem="d_c")
            for k in range(8):
                wb = wada[k % 2]
                R.dma("sp", lambda e, k=k, wb=wb, c0=c0: e.dma_start(out=wb[:, 0:1536], in_=w_ada[:, k, c0:c0 + 1536]), w=(f"wada{k % 2}",),
                      sem=f"d_wada{k % 2}")
                R.dma("act", lambda e, k=k, wb=wb, c0=c0: e.dma_start(out=wb[:, 1536:3072], in_=w_ada[:, k, c0 + 1536:c0 + 3072]),
                      w=(f"wada{k % 2}",), sem=f"d_wada{k % 2}")

                def mm(e, k=k, wb=wb):
                    ins = None
                    for j in range(6):
                        ins = e.matmul(ps[j][:, :], lhsT=csB[:, k, :], rhs=wb[:, j * 512:(j + 1) * 512], start=(k == 0), stop=(k == 7))
                    return ins
                R.op("pe", mm, r=(f"wada{k % 2}", f"csB{k}"), w=tuple(f"ps{j}" for j in range(6)))
            for j in range(2):
                sl = slice(j * 512, (j + 1) * 512)
                R.op("dve", lambda e, j=j, sl=sl, Brow=Brow: e.tensor_tensor(out=Brow[:, sl], in0=ps[j][:, :], in1=brow[:, sl], op=ALU.add),
                     r=(f"ps{j}", "brow"), w=(f"Brow{pas}",))
                R.op("dve", lambda e, j=j, sl=sl: e.tensor_tensor(out=t1[:, sl], in0=ps[2 + j][:, :], in1=brow[:, 1024 + j * 512:1024 + (j + 1) * 512],
                                                                  op=ALU.add), r=(f"ps{2 + j}", "brow"), w=("t1m",))
                R.op("dve", lambda e, sl=sl, Arow=Arow: e.scalar_tensor_tensor(out=Arow[:, sl], in0=t1[:, sl], scalar=1.0, in1=g1[:, sl],
                                                                              op0=ALU.add, op1=ALU.mult), r=("t1m", "g1"), w=(f"Arow{pas}",))
                R.op("dve", lambda e, j=j, sl=sl: e.tensor_tensor(out=t1[:, sl], in0=ps[4 + j][:, :], in1=brow[:, 2048 + j * 512:2048 + (j + 1) * 512],
                                                                  op=ALU.add), r=(f"ps{4 + j}", "brow", f"Arow{pas}"), w=("t1m",))
                R.op("dve", lambda e, sl=sl, Grow=Grow: e.tensor_tensor(out=Grow[:, sl], in0=t1[:, sl], in1=g2[:, sl], op=ALU.mult),
                     r=("t1m", "g2"), w=(f"Grow{pas}",))
        dump("Arow_m", Arow_m, [128, 1024], ("Arow0",))
        dump("Gf", Gf, [128, 1024], ("Grow1",))
        R.flush()
    if stop_after <= 0:
        return finish()

    hT = sb("hT", [128, 8, 4096], BF16, ar="A")
    with Mark():
        xt = [sb(f"xt{i}", [128, 1024]) for i in range(4)]
        hm = [sb(f"hm{i}", [128, 1024]) for i in range(4)]
        junk = sb("junk", [128, 1024], BF16)
        ss = sb("ss", [128, 32])
        tmpv = sb("tmpv", [128, 32])
        sdv = sb("sdv", [128, 32])
        rstd = sb("rstd", [128, 32])
        def p1_stageA(i):
            b = i % 4
            R.dma("sp" if i % 2 == 0 else "act", lambda e, i=i, b=b: e.dma_start(out=xt[b], in_=xh[i * 128:(i + 1) * 128, :]),
                  w=(f"xt{b}",), sem=f"d_xt{b}")
            R.op("act", lambda e, i=i, b=b: e.activation(out=junk, in_=xt[b], func=AF.Square, accum_out=ss[:, i:i + 1]),
                 r=(f"xt{b}",), w=("junk", f"ss{i}"))
            R.op("dve", lambda e, i=i: e.tensor_scalar(out=tmpv[:, i:i + 1], in0=ss[:, i:i + 1], scalar1=1.0 / 1024, scalar2=EPS,
                                                       op0=ALU.mult, op1=ALU.add), r=(f"ss{i}",), w=(f"tmpv{i}",))
            R.op("act", lambda e, i=i: e.activation(out=sdv[:, i:i + 1], in_=tmpv[:, i:i + 1], func=AF.Sqrt), r=(f"tmpv{i}",), w=(f"sdv{i}",))
            R.op("dve", lambda e, i=i: e.reciprocal(out=rstd[:, i:i + 1], in_=sdv[:, i:i + 1]), r=(f"sdv{i}",), w=(f"rstd{i}",))
            R.op("dve", lambda e, i=i, b=b: e.scalar_tensor_tensor(out=hm[b], in0=xt[b], scalar=rstd[:, i:i + 1], in1=Arow_m,
                                                                   op0=ALU.mult, op1=ALU.mult), r=(f"xt{b}", f"rstd{i}"), w=(f"hm{b}",))
            R.op("pool", lambda e, b=b: e.tensor_tensor(out=hm[b], in0=hm[b], in1=Brow_m, op=ALU.add), r=(f"hm{b}",), w=(f"hm{b}",))
            pb = (i % 4) * 2

            def mm(e, b=b, pb=pb):
                ins = None
                for k in range(8):
                    ins = e.matmul(ps[pb + k // 4][:, (k % 4) * 128:(k % 4 + 1) * 128], lhsT=hm[b][:, k * 128:(k + 1) * 128],
                                   rhs=ident[:], start=True, stop=True)
                return ins
            R.op("pe", mm, r=(f"hm{b}", "ident"), w=(f"ps{pb}", f"ps{pb + 1}"))

        def p1_stageB(i):
            pb = (i % 4) * 2
            R.op("act", lambda e, i=i, pb=pb: e.activation(out=hT[:, 0:4, i * 128:(i + 1) * 128],
                                                           in_=ps[pb][:, :].rearrange("p (k t) -> p k t", k=4), func=AF.Copy),
                 r=(f"ps{pb}",), w=(f"hT{i}a",))
            R.op("dve", lambda e, i=i, pb=pb: e.tensor_copy(out=hT[:, 4:8, i * 128:(i + 1) * 128],
                                                            in_=ps[pb + 1][:, :].rearrange("p (k t) -> p k t", k=4)),
                 r=(f"ps{pb + 1}",), w=(f"hT{i}b",))

        p1_stageA(0)
        p1_stageA(1)
        for i in range(NTH):
            p1_stageB(i)
            if i + 2 < NTH:
                p1_stageA(i + 2)
        dump("hT", hT[:, :, 1024:1024 + 256], [128, 8, 256], [f"hT{i}{c}" for i in (8, 9) for c in "ab"], BF16)
        R.flush()
    top["B"] = baseB
    if stop_after <= 1:
        return finish()

    with Mark():
        attn_tm = sb("attn_tm", [128, 16, 1024], BF16)
        Wp = [sb(f"Wp{i}", [128, 8, 3, 128], BF16) for i in range(2)]
        QT = [sb(f"QT{i}", [128, 2048], BF16) for i in range(2)]
        KT = [sb(f"KT{i}", [128, 4096], BF16) for i in range(2)]
        Vx = [sb(f"Vx{i}", [128, 32, 2, 65], BF16) for i in range(2)]
        Eb = [sb(f"Eb{i}", [128, 17 * 128], BF16) for i in range(2)]
        exb = [sb(f"exb{i}", [128, 512], BF16) for i in range(4)]
        PT = [sb(f"PT{i}", [128, 512], BF16) for i in range(4)]
        rc = [sb(f"rc{i}", [128, 8]) for i in range(2)]
        for b in range(2):
            for a in range(2):
                R.op("dve", lambda e, b=b, a=a: e.tensor_copy(out=Vx[b][:, :, a, 64], in_=valid[:]), r=("valid",), w=(f"Vx{b}c",))
        nexc = [0]
        for hp in range(8):
            b = hp % 2
            for j in range(3):
                R.dma("pool", lambda e, b=b, j=j, hp=hp: e.dma_start(out=Wp[b][:, :, j, :],
                                                                      in_=w_in[:, :, j * 1024 + hp * 128:j * 1024 + (hp + 1) * 128]),
                      w=(f"Wp{b}",), sem=f"d_Wp{b}")
            for tb in range(4):
                pk = tb % 4

                def mm(e, b=b, tb=tb, pk=pk):
                    ins = None
                    for k in range(8):
                        ins = e.matmul(ps[pk][:, :], lhsT=Wp[b][:, k, 0, :], rhs=hT[:, k, 1024 + tb * 512:1024 + (tb + 1) * 512],
                                       start=(k == 0), stop=(k == 7))
                    return ins
                R.op("pe", mm, r=(f"Wp{b}",), w=(f"ps{pk}",))
                R.op("act", lambda e, b=b, tb=tb, pk=pk: e.activation(out=QT[b][:, tb * 512:(tb + 1) * 512], in_=ps[pk][:, :],
                                                                      func=AF.Copy, scale=0.125), r=(f"ps{pk}",), w=(f"QT{b}",))
            for tb in range(8):
                pk = tb % 4

                def mm(e, b=b, tb=tb, pk=pk):
                    ins = None
                    for k in range(8):
                        ins = e.matmul(ps[pk][:, :], lhsT=Wp[b][:, k, 1, :], rhs=hT[:, k, tb * 512:(tb + 1) * 512],
                                       start=(k == 0), stop=(k == 7))
                    return ins
                R.op("pe", mm, r=(f"Wp{b}",), w=(f"ps{pk}",))
                if tb % 2:
                    R.op("dve", lambda e, b=b, tb=tb, pk=pk: e.tensor_copy(out=KT[b][:, tb * 512:(tb + 1) * 512], in_=ps[pk][:, :]),
                         r=(f"ps{pk}",), w=(f"KT{b}",))
                else:
                    R.op("act", lambda e, b=b, tb=tb, pk=pk: e.activation(out=KT[b][:, tb * 512:(tb + 1) * 512], in_=ps[pk][:, :],
                                                                          func=AF.Copy), r=(f"ps{pk}",), w=(f"KT{b}",))
            for i4 in range(8):
                pk = i4 % 4

                def mm(e, b=b, i4=i4, pk=pk):
                    ins = None
                    for ii in range(4):
                        i = i4 * 4 + ii
                        for k in range(8):
                            ins = e.matmul(ps[pk][:, ii * 128:(ii + 1) * 128], lhsT=hT[:, k, i * 128:(i + 1) * 128],
                                           rhs=Wp[b][:, k, 2, :], start=(k == 0), stop=(k == 7))
                    return ins
                R.op("pe", mm, r=(f"Wp{b}",), w=(f"ps{pk}",))
                for ii in range(4):
                    i = i4 * 4 + ii
                    R.op("dve", lambda e, b=b, i=i, ii=ii, pk=pk: e.tensor_scalar(
                        out=Vx[b][:, i, :, 0:64], in0=ps[pk][:, ii * 128:(ii + 1) * 128].rearrange("p (a e) -> p a e", a=2),
                        scalar1=valid[:, i:i + 1], scalar2=None, op0=ALU.mult), r=(f"ps{pk}", "valid"), w=(f"Vx{b}",))
            if hp == 0:
                dump("QT", QT[0], [128, 2048], ("QT0",), BF16)
                dump("KT", KT[0], [128, 4096], ("KT0",), BF16)
            for a in range(2):
                h = 2 * hp + a
                eb = h % 2
                R.dma("pool", lambda e, h=h, eb=eb: e.dma_start(out=Eb[eb], in_=etab_d[h]), w=(f"Eb{eb}",), sem=f"d_Eb{eb}")
                rows = slice(a * 64, (a + 1) * 64)
                groups = []
                for qi in range(16):
                    for o0 in (0, 4, 8, 12, 16):
                        groups.append((qi, o0, min(4, 17 - o0)))

                def emit_S(gidx, b=b, rows=rows, groups=groups):
                    qi, o0, n = groups[gidx]
                    pk = gidx % 4

                    def mm(e, qi=qi, o0=o0, n=n, pk=pk, b=b, rows=rows):
                        ins = None
                        for j in range(n):
                            ins = e.matmul(ps[pk][:, j * 128:(j + 1) * 128], lhsT=KT[b][rows, (qi + o0 + j) * 128:(qi + o0 + j + 1) * 128],
                                           rhs=QT[b][rows, qi * 128:(qi + 1) * 128], start=True, stop=True)
                        return ins
                    R.op("pe", mm, r=(f"KT{b}", f"QT{b}"), w=(f"ps{pk}",))

                def emit_rest(gidx, b=b, a=a, h=h, eb=eb, groups=groups):
                    qi, o0, n = groups[gidx]
                    pk = gidx % 4
                    x3 = nexc[0] % 4
                    nexc[0] += 1
                    po = 6 + qi % 2
                    R.op("act", lambda e, n=n, pk=pk, x3=x3: e.activation(out=exb[x3][:, 0:n * 128], in_=ps[pk][:, 0:n * 128], func=AF.Exp),
                         r=(f"ps{pk}",), w=(f"exb{x3}",))
                    R.op("dve", lambda e, n=n, o0=o0, x3=x3, eb=eb: e.tensor_tensor(out=PT[x3][:, 0:n * 128], in0=exb[x3][:, 0:n * 128],
                                                                                    in1=Eb[eb][:, o0 * 128:(o0 + n) * 128], op=ALU.mult),
                         r=(f"exb{x3}", f"Eb{eb}"), w=(f"PT{x3}",))

                    def mm(e, qi=qi, o0=o0, n=n, x3=x3, po=po, b=b, a=a):
                        ins = None
                        for j in range(n):
                            o = o0 + j
                            ins = e.matmul(ps[po][:, 0:65], lhsT=PT[x3][:, j * 128:(j + 1) * 128], rhs=Vx[b][:, qi + o, a, :],
                                           start=(o == 0), stop=(o == 16))
                        return ins
                    R.op("pe", mm, r=(f"PT{x3}", f"Vx{b}", f"Vx{b}c"), w=(f"ps{po}",))
                    if o0 == 16:
                        r2 = qi % 2
                        R.op("dve", lambda e, po=po, r2=r2: e.reciprocal(out=rc[r2][:, 0:1], in_=ps[po][:, 64:65]), r=(f"ps{po}",), w=(f"rc{r2}",))
                        R.op("dve", lambda e, po=po, r2=r2, qi=qi, h=h: e.tensor_scalar(
                            out=attn_tm[:, qi, h * 64:(h + 1) * 64], in0=ps[po][:, 0:64], scalar1=rc[r2][:, 0:1], scalar2=None,
                            op0=ALU.mult), r=(f"ps{po}", f"rc{r2}"), w=(f"attn{qi}",))
                LOOK = 3
                for g0 in range(LOOK):
                    emit_S(g0)
                for gidx in range(len(groups)):
                    if gidx + LOOK < len(groups):
                        emit_S(gidx + LOOK)
                    emit_rest(gidx)
        dump("attn0", attn_tm[:, 0, :], [128, 1024], ("attn0",), BF16)
        dump("attn15", attn_tm[:, 15, :], [128, 1024], ("attn15",), BF16)
        R.dma("sp", lambda e: e.dma_start(out=attn_d.rearrange("(i p) f -> p i f", p=128), in_=attn_tm),
              r=tuple(f"attn{q}" for q in range(16)), w=("attn_d",), sem="d_spill")
        R.flush()
    with Mark():
        zs_tm = sb("zs_tm", [128, 16, 1024], BF16)
        Wz = sb("Wz", [128, 8, 1024], BF16)
        R.dma("pool", lambda e: e.dma_start(out=Wz, in_=w_in[:, :, 3072:4096]), w=("Wz",), sem="d_w2")
        for i in range(16):
            for hf in range(2):
                pk = (2 * i + hf) % 4

                def mm(e, i=i, hf=hf, pk=pk):
                    ins = None
                    for k in range(8):
                        ins = e.matmul(ps[pk][:, :], lhsT=hT[:, k, (8 + i) * 128:(9 + i) * 128], rhs=Wz[:, k, hf * 512:(hf + 1) * 512],
                                       start=(k == 0), stop=(k == 7))
                    return ins
                R.op("pe", mm, r=("Wz",), w=(f"ps{pk}",))
                R.op("act", lambda e, i=i, hf=hf, pk=pk: e.activation(out=zs_tm[:, i, hf * 512:(hf + 1) * 512], in_=ps[pk][:, :],
                                                                      func=AF.Silu), r=(f"ps{pk}",), w=(f"zs{i}",))
        R.dma("sp", lambda e: e.dma_start(out=zs_d.rearrange("(i p) f -> p i f", p=128), in_=zs_tm),
              r=tuple(f"zs{q}" for q in range(16)), w=("zs_d",), sem="d_spill")
        R.flush()
    if stop_after <= 2:
        return finish()

    X_tm = sb("X_tm", [128, 16, 1280], BF16)
    BC_fm = sb("BC_fm", [128, 4, 2048], BF16)
    dt_t = sb("dt_t", [128, 16, 32])
    adt = sb("adt", [128, 16, 32])
    eac = sb("eac", [128, 16, 32])
    eEnd = sb("eEnd", [128, 16, 32])
    cdec = sb("cdec", [128, 16, 32])
    dskip = sb("dskip", [128, 16])
    Hloc = [sb(f"Hloc{d}", [128, 1024]) for d in range(2)]
    with Mark():
        Wc = [sb(f"Wc{i}", [128, 8, 128], BF16) for i in range(2)]
        Wdt = sb("Wdt", [128, 8, 32], BF16)
        xsTc = [sb(f"xsTc{i}", [128, 2048], BF16) for i in range(2)]
        craw = sb("craw", [128, 2056])
        cacc = sb("cacc", [128, 2048])
        convw = sb("convw", [128, 12, 5])
        convb = sb("convb", [128, 12])
        dtb = sb("dtb", [128, 32])
        abc = sb("abc", [128, 32])
        dtr = sb("dtr", [128, 16, 32])
        wde = sb("wde", [128, 16, 32])
        xw = [sb(f"xw{i}", [128, 1024], BF16) for i in range(2)]
        bnc_sb = sb("bnc_sb", [128, 32])
        R.dma("pool", lambda e: e.dma_start(out=Wdt, in_=w_in[:, :, 5632:5664]), w=("Wdt",), sem="d_w2")
        R.dma("sp", lambda e: e.dma_start(out=convw, in_=convw_d.rearrange("p (a b) -> p a b", a=12)), w=("convw",), sem="d_c")
        R.dma("sp", lambda e: e.dma_start(out=convb, in_=convb_d), w=("convb",), sem="d_c")
        R.dma("sp", lambda e: e.dma_start(out=dtb, in_=dtb_row.partition_broadcast(128)), w=("dtb",), sem="d_c")
        R.dma("sp", lambda e: e.dma_start(out=abc, in_=alog_row.partition_broadcast(128)), w=("abc0",), sem="d_c")
        R.dma("sp", lambda e: e.dma_start(out=dskip, in_=dskip_row.partition_broadcast(128)), w=("dskip",), sem="d_c")
        R.op("act", lambda e: e.activation(out=abc, in_=abc, func=AF.Exp), r=("abc0",), w=("abc1",))
        R.op("dve", lambda e: e.tensor_scalar(out=abc, in0=abc, scalar1=-1.0, scalar2=None, op0=ALU.mult), r=("abc1",), w=("abc",))
        pb5 = ps[5][:, :].bitcast(BF16)
        pb6 = ps[6][:, :].bitcast(BF16)
        for c in range(12):
            wb = c % 2
            R.dma("pool", lambda e, c=c, wb=wb: e.dma_start(out=Wc[wb], in_=w_in[:, :, 4096 + c * 128:4096 + (c + 1) * 128]),
                  w=(f"Wc{wb}",), sem=f"d_Wc{wb}")
            for blk in range(4):
                def mm(e, wb=wb, blk=blk):
                    ins = None
                    for k in range(8):
                        ins = e.matmul(ps[blk][:, :], lhsT=Wc[wb][:, k, :], rhs=hT[:, k, 1024 + blk * 512:1024 + (blk + 1) * 512],
                                       start=(k == 0), stop=(k == 7))
                    return ins
                R.op("pe", mm, r=(f"Wc{wb}",), w=(f"ps{blk}",))

            def mmh(e, wb=wb, c=c):
                ins = None
                for side, t0 in ((0, 1022), (1, 3072)):
                    for k in range(8):
                        ins = e.matmul(ps[4][:, c * 4 + side * 2:c * 4 + side * 2 + 2], lhsT=Wc[wb][:, k, :], rhs=hT[:, k, t0:t0 + 2],
                                       start=(k == 0), stop=(k == 7))
                return ins
            R.op("pe", mmh, r=(f"Wc{wb}",), w=("ps4",))
            for blk in range(4):
                dst = craw[:, 2 + blk * 512:2 + (blk + 1) * 512]
                if blk < 2:
                    R.op("act", lambda e, blk=blk, dst=dst: e.activation(out=dst, in_=ps[blk][:, :], func=AF.Copy),
                         r=(f"ps{blk}",), w=(f"craw{blk}",))
                else:
                    R.op("dve", lambda e, blk=blk, dst=dst: e.tensor_copy(out=dst, in_=ps[blk][:, :]), r=(f"ps{blk}",), w=(f"craw{blk}",))
            R.op("dve", lambda e, c=c: e.tensor_scalar(out=craw[:, 0:2], in0=ps[4][:, c * 4:c * 4 + 2], scalar1=edge[:, 0:1], scalar2=None,
                                                       op0=ALU.mult), r=("ps4", "edge"), w=("crawh0",))
            R.op("dve", lambda e, c=c: e.tensor_scalar(out=craw[:, 2050:2052], in0=ps[4][:, c * 4 + 2:c * 4 + 4], scalar1=edge[:, 1:2],
                                                       scalar2=None, op0=ALU.mult), r=("ps4", "edge"), w=("crawh1",))
            ck = [f"craw{j}" for j in range(4)] + ["crawh0", "crawh1"]
            R.op("dve", lambda e, c=c: e.tensor_scalar(out=cacc, in0=craw[:, 0:2048], scalar1=convw[:, c, 0:1], scalar2=None, op0=ALU.mult),
                 r=ck + ["convw"], w=("cacc",))
            for i in range(1, 5):
                R.op("dve", lambda e, c=c, i=i: e.scalar_tensor_tensor(out=cacc, in0=craw[:, i:i + 2048], scalar=convw[:, c, i:i + 1], in1=cacc,
                                                                       op0=ALU.mult, op1=ALU.add), r=ck + ["cacc"], w=("cacc",))
            if c < 8:
                dst, dkey = xsTc[c % 2], f"xsTc{c % 2}"
            else:
                dst, dkey = BC_fm[:, c - 8, :], f"BCfm{c - 8}"
            R.op("act", lambda e, c=c, dst=dst: e.activation(out=dst, in_=cacc, func=AF.Silu, bias=convb[:, c:c + 1]),
                 r=("cacc", "convb"), w=(dkey,))
            if c == 0:
                dump("xbcT0", xsTc[0], [128, 2048], ("xsTc0",), BF16)
            if c == 9:
                dump("xbcT9", BC_fm[:, 1, :], [128, 2048], ("BCfm1",), BF16)
            if c < 10:
                def tr(e, dst=dst):
                    ins = None
                    for i in range(16):
                        pbv = pb5 if i < 8 else pb6
                        ins = e.transpose(out=pbv[:, (i % 8) * 128:(i % 8 + 1) * 128], in_=dst[:, i * 128:(i + 1) * 128], identity=identb[:])
                    return ins
                R.op("pe", tr, r=(dkey, "identb"), w=("ps5", "ps6"))
                R.op("act", lambda e, c=c: e.activation(out=X_tm[:, 0:8, c * 128:(c + 1) * 128],
                                                        in_=pb5[:, 0:1024].rearrange("p (i f) -> p i f", i=8), func=AF.Copy),
                     r=("ps5",), w=(f"Xtm_{c}a",))
                R.op("dve", lambda e, c=c: e.tensor_copy(out=X_tm[:, 8:16, c * 128:(c + 1) * 128],
                                                         in_=pb6[:, 0:1024].rearrange("p (i f) -> p i f", i=8)),
                     r=("ps6",), w=(f"Xtm_{c}b",))
        xkeys = [f"Xtm_{c}{s}" for c in range(10) for s in "ab"]
        dump("Xtm3", X_tm[:, 3, :], [128, 1280], xkeys, BF16)

        def mmdt(e):
            ins = None
            for i in range(16):
                for k in range(8):
                    ins = e.matmul(ps[7][:, i * 32:(i + 1) * 32], lhsT=hT[:, k, (8 + i) * 128:(9 + i) * 128], rhs=Wdt[:, k, :],
                                   start=(k == 0), stop=(k == 7))
            return ins
        R.op("pe", mmdt, r=("Wdt",), w=("ps7",))
        R.op("dve", lambda e: e.tensor_tensor(out=dtr, in0=ps[7][:, :].rearrange("p (c h) -> p c h", c=16),
                                              in1=dtb.unsqueeze(1).to_broadcast([128, 16, 32]), op=ALU.add), r=("ps7", "dtb"), w=("dtr",))
        R.op("act", lambda e: e.activation(out=dtr, in_=dtr, func=AF.Exp), r=("dtr",), w=("dtr",))
        R.op("act", lambda e: e.activation(out=dt_t, in_=dtr, func=AF.Ln, bias=1.0), r=("dtr",), w=("dt_t",))
        R.op("dve", lambda e: e.tensor_tensor(out=adt, in0=dt_t, in1=abc.unsqueeze(1).to_broadcast([128, 16, 32]), op=ALU.mult),
             r=("dt_t", "abc"), w=("adt",))
        dump("dt", dt_t, [128, 16, 32], ("dt_t",))
        adt2 = adt.rearrange("p c h -> p (c h)")
        for j, m in enumerate((triLE, triGE, mSU, mSL, ones)):
            R.op("pe", lambda e, j=j, m=m: e.matmul(ps[j][:, :], lhsT=m, rhs=adt2, start=True, stop=True), r=("adt", "masks"), w=(f"ps{j}",))

        def pv3(j):
            return ps[j][:, :].rearrange("p (c h) -> p c h", c=16)
        for d in range(2):
            hs = slice(d * 16, (d + 1) * 16)
            R.op("act", lambda e, d=d, hs=hs: e.activation(out=eac[:, :, hs], in_=pv3(d)[:, :, hs], func=AF.Exp), r=(f"ps{d}",), w=(f"eac{d}",))
            R.op("act", lambda e, d=d, hs=hs: e.activation(out=eEnd[:, :, hs], in_=pv3(2 + d)[:, :, hs], func=AF.Exp), r=(f"ps{2 + d}",),
                 w=(f"eEnd{d}",))
        R.op("act", lambda e: e.activation(out=cdec, in_=pv3(4), func=AF.Exp), r=("ps4",), w=("cdec",))
        R.op("dve", lambda e: e.tensor_copy(out=bnc_sb, in_=cdec[:, 0, :]), r=("cdec",), w=("bnc_sb",))
        for c in range(1, 16):
            R.op("dve", lambda e, c=c: e.tensor_tensor(out=bnc_sb, in0=bnc_sb, in1=cdec[:, c, :], op=ALU.mult), r=("bnc_sb", "cdec"), w=("bnc_sb",))

        R.op("dve", lambda e: e.tensor_tensor(out=wde, in0=dt_t, in1=eEnd, op=ALU.mult), r=("dt_t", "eEnd0", "eEnd1"), w=("wde",))
        for d in range(2):
            R.op("pool", lambda e, d=d: e.memset(Hloc[d], 0.0), w=(f"H{d}",))
            order = range(16) if d == 0 else range(15, -1, -1)
            for n_i, c in enumerate(order):
                b = n_i % 2
                R.op("dve", lambda e, c=c, d=d, b=b: e.tensor_tensor(
                    out=v3(xw[b]), in0=v3(X_tm[:, c, 0:1024]),
                    in1=wde[:, c, d * 16:(d + 1) * 16].unsqueeze(2).to_broadcast([128, 16, 64]), op=ALU.mult),
                    r=xkeys + ["wde"], w=(f"xw{b}",))
                pp = 2 * b

                def mms(e, c=c, b=b, pp=pp):
                    ins = None
                    for g in range(2):
                        ins = e.matmul(ps[pp + g][:, :], lhsT=X_tm[:, c, 1024 + g * 128:1024 + (g + 1) * 128],
                                       rhs=xw[b][:, g * 512:(g + 1) * 512], start=True, stop=True)
                    return ins
                R.op("pe", mms, r=[f"xw{b}"] + xkeys, w=(f"ps{pp}", f"ps{pp + 1}"))
                R.op("dve", lambda e, c=c, d=d: e.tensor_tensor(
                    out=v3(Hloc[d]), in0=v3(Hloc[d]),
                    in1=cdec[:, c, d * 16:(d + 1) * 16].unsqueeze(2).to_broadcast([128, 16, 64]), op=ALU.mult),
                    r=(f"H{d}", "cdec"), w=(f"H{d}",))
                for g in range(2):
                    R.op("dve", lambda e, d=d, g=g, pp=pp: e.tensor_tensor(
                        out=Hloc[d][:, g * 512:(g + 1) * 512], in0=Hloc[d][:, g * 512:(g + 1) * 512], in1=ps[pp + g][:, :],
                        op=ALU.add), r=(f"H{d}", f"ps{pp + g}"), w=(f"H{d}",))
        dump("Hloc0", Hloc[0], [128, 1024], ("H0",))
        dump("Hloc1", Hloc[1], [128, 1024], ("H1",))
        for d in range(2):
            R.dma("sp", lambda e, d=d: e.dma_start(out=bnc_in[:, d * 1024:(d + 1) * 1024], in_=Hloc[d]), r=(f"H{d}",),
                  w=("bnc_in",), sem="d_bnc")
        R.dma("sp", lambda e: e.dma_start(out=bnc2_in[:, 0:32], in_=bnc_sb), r=("bnc_sb",), w=("bnc2_in",), sem="d_bnc")
        R.dma("sp", lambda e: e.dma_start(out=bnc2_in[:, 32:64], in_=bnc_sb), r=("bnc_sb",), w=("bnc2_in",), sem="d_bnc")
        R.op("pool", lambda e: e.collective_compute("AllGather", ALU.bypass, replica_groups=[[0, 1, 2, 3], [4, 5, 6, 7]],
                                                   ins=[bnc_in], outs=[bnc_out]),
             r=("bnc_in",), w=("bnc_out",), semkey="cc", inc=None)
        R.op("pool", lambda e: e.collective_compute("AllGather", ALU.bypass, replica_groups=[[0, 1, 2, 3], [4, 5, 6, 7]],
                                                   ins=[bnc2_in], outs=[bnc2_out]),
             r=("bnc2_in",), w=("bnc2_out",), semkey="cc", inc=None)
        R.op("pool", lambda e: e.memset(bnc_sb[:, 0:1], 0.0), r=("bnc_out", "bnc2_out", "bnc2_in"), w=("ccdone",))
        R.flush()
    if stop_after <= 3:
        return finish()

    top["A"] = 0
    yacc = sb("yacc", [128, 16, 1024], BF16, ar="A")
    Hin = [sb(f"Hin{d}", [128, 1024], ar="A") for d in range(2)]
    with Mark():
        gath = sb("gath", [128, 4, 2048])
        gath2 = sb("gath2", [128, 4, 64])
        Pc = sb("Pc", [128, 1024])
        R.dma("sp", lambda e: e.dma_start(out=gath, in_=bnc_out.rearrange("(m p) n -> p m n", p=128)), r=("bnc_out",), w=("gath",), sem="d_g")
        R.dma("sp", lambda e: e.dma_start(out=gath2, in_=bnc2_out.rearrange("(m p) n -> p m n", p=128)), r=("bnc2_out",), w=("gath",), sem="d_g")
        for d in range(2):
            R.op("pool", lambda e, d=d: e.memset(Hin[d], 0.0), w=(f"Hin{d}",))
            R.op("pool", lambda e: e.memset(Pc, 0.0), w=("Pc",))
            seq = [0, 1, 2] if d == 0 else [3, 2, 1]
            for m in seq:
                tgt = m + 1 if d == 0 else m - 1
                R.op("dve", lambda e, m=m, d=d: e.tensor_tensor(
                    out=v3(Pc), in0=v3(Pc), in1=gath2[:, m, d * 16:(d + 1) * 16].unsqueeze(2).to_broadcast([128, 16, 64]),
                    op=ALU.mult), r=("Pc", "gath"), w=("Pc",))
                R.op("dve", lambda e, m=m, d=d: e.tensor_tensor(out=Pc, in0=Pc, in1=gath[:, m, d * 1024:(d + 1) * 1024], op=ALU.add),
                     r=("Pc", "gath"), w=("Pc",))
                R.op("dve", lambda e, d=d, tgt=tgt: e.scalar_tensor_tensor(out=Hin[d], in0=Pc, scalar=sel[:, tgt:tgt + 1], in1=Hin[d],
                                                                          op0=ALU.mult, op1=ALU.add), r=("Pc", "sel", f"Hin{d}"), w=(f"Hin{d}",))
        dump("Hin0", Hin[0], [128, 1024], ("Hin0",))
        dump("Hin1", Hin[1], [128, 1024], ("Hin1",))
        R.flush()
    with Mark(), Mark("A"):
        ytmp = [sb(f"ytmp{i}", [128, 1024], ar="A") for i in range(2)]
        ytot = [sb(f"ytot{i}", [128, 1024], ar="A") for i in range(2)]
        exs = [sb(f"exs{i}", [128, 16, 128], BF16, ar="A") for i in range(2)]
        Rm = [sb(f"Rm{i}", [128, 16, 128]) for i in range(2)]
        MT = [sb(f"MT{i}", [128, 16, 128], BF16) for i in range(2)]
        CBm = [sb(f"CBm{i}", [128, 2, 128], BF16) for i in range(2)]
        xd = [sb(f"xd{i}", [128, 1024], BF16) for i in range(2)]
        xw2 = [sb(f"xw2{i}", [128, 1024], BF16) for i in range(2)]
        Hb = sb("Hb", [128, 1024], BF16)
        gsq = sb("gsq", [128, 1024], BF16)
        wde2 = sb("wde2", [128, 16, 32])
        ssg = sb("ssg", [128, 16])
        tg = sb("tg", [128, 16])
        sg_ = sb("sg_", [128, 16])
        rg = sb("rg", [128, 16])
        zsb = [sb(f"zsb{i}", [128, 1024], BF16) for i in range(2)]
        R.op("dve", lambda e: e.tensor_tensor(out=wde2, in0=dt_t, in1=eEnd, op=ALU.mult), w=("wde2",))
        for d in range(2):
            hs = slice(d * 16, (d + 1) * 16)
            tri = triLE if d == 0 else triGE
            sm = mSU if d == 0 else mSL
            H = Hin[d]
            R.op("act", lambda e, H=H: e.activation(out=Hb, in_=H, func=AF.Copy), r=(f"Hin{d}",), w=("Hb",))
            order = list(range(16)) if d == 0 else list(range(15, -1, -1))

            def front(n_i, d=d, hs=hs, tri=tri, sm=sm, order=order):
                c = order[n_i]
                p = n_i % 2
                cs_ = slice(c * 128, (c + 1) * 128)
                if d == 1:
                    R.dma("sp", lambda e: e.dma_start(out=zsb[p], in_=zs_d[c * 128:(c + 1) * 128, :]), r=("zs_d",),
                          w=(f"zsb{p}",), sem=f"d_zs{p}")

                def mmcb(e):
                    ins = None
                    for g in range(2):
                        ins = e.matmul(ps[0][:, g * 128:(g + 1) * 128], lhsT=BC_fm[:, g, cs_], rhs=BC_fm[:, 2 + g, cs_], start=True, stop=True)
                    return ins
                R.op("pe", mmcb, w=("ps0",))
                R.op("dve", lambda e: e.tensor_tensor(out=CBm[p], in0=ps[0][:, 0:256].rearrange("p (g l) -> p g l", g=2),
                                                      in1=tri.unsqueeze(1).to_broadcast([128, 2, 128]), op=ALU.mult),
                     r=("ps0",), w=(f"CBm{p}",))
                R.op("pool", lambda e: e.tensor_tensor(
                    out=Rm[p], in0=adt[:, c, hs].unsqueeze(2).to_broadcast([128, 16, 128]),
                    in1=tri.unsqueeze(1).to_broadcast([128, 16, 128]), op=ALU.mult), w=(f"Rm{p}",))
                Rm2 = Rm[p].rearrange("p h l -> p (h l)")

                def mmseg(e):
                    ins = None
                    for j in range(4):
                        ins = e.matmul(ps[1 + j][:, :], lhsT=sm, rhs=Rm2[:, j * 512:(j + 1) * 512], start=True, stop=True)
                    return ins
                R.op("pe", mmseg, r=(f"Rm{p}",), w=("ps1", "ps2", "ps3", "ps4"))
                for j in range(4):
                    R.op("act", lambda e, j=j: e.activation(out=exs[p][:, j * 4:(j + 1) * 4, :],
                                                            in_=ps[1 + j][:, :].rearrange("p (h l) -> p h l", h=4), func=AF.Exp),
                         r=(f"ps{1 + j}",), w=(f"exs{p}_{j}",))
                R.op("dve", lambda e: e.tensor_tensor(out=v3(xd[p]), in0=v3(X_tm[:, c, 0:1024]),
                                                      in1=dt_t[:, c, hs].unsqueeze(2).to_broadcast([128, 16, 64]), op=ALU.mult),
                     w=(f"xd{p}",))
                R.op("dve", lambda e: e.tensor_tensor(out=v3(xw2[p]), in0=v3(X_tm[:, c, 0:1024]),
                                                      in1=wde2[:, c, hs].unsqueeze(2).to_broadcast([128, 16, 64]), op=ALU.mult),
                     r=("wde2",), w=(f"xw2{p}",))
                for g in range(2):
                    R.op("dve", lambda e, g=g: e.tensor_tensor(out=MT[p][:, g * 8:(g + 1) * 8, :], in0=exs[p][:, g * 8:(g + 1) * 8, :],
                                                              in1=CBm[p][:, g:g + 1, :].to_broadcast([128, 8, 128]), op=ALU.mult),
                         r=(f"exs{p}_{2 * g}", f"exs{p}_{2 * g + 1}", f"CBm{p}"), w=(f"MT{p}_{g}",))

            def tail(n_i, d=d, hs=hs, H=H, order=order):
                c = order[n_i]
                p = n_i % 2
                cs_ = slice(c * 128, (c + 1) * 128)

                def mmy(e):
                    ins = None
                    for h in range(16):
                        ins = e.matmul(ps[5 + h // 8][:, (h % 8) * 64:(h % 8 + 1) * 64], lhsT=MT[p][:, h, :], rhs=xd[p][:, h * 64:(h + 1) * 64],
                                       start=True, stop=True)
                    return ins
                R.op("pe", mmy, r=(f"MT{p}_0", f"MT{p}_1", f"xd{p}"), w=("ps5", "ps6"))
                for g in range(2):
                    gs = slice(g * 512, (g + 1) * 512)
                    R.op("pe", lambda e, g=g: e.matmul(ps[7][:, :], lhsT=BC_fm[:, 2 + g, cs_], rhs=Hb[:, g * 512:(g + 1) * 512], start=True, stop=True),
                         r=("Hb",), w=("ps7",))
                    R.op("dve", lambda e, g=g, gs=gs: e.tensor_tensor(
                        out=v3(ytmp[p][:, gs]), in0=v3(ps[7][:, :]),
                        in1=eac[:, c, d * 16 + g * 8:d * 16 + (g + 1) * 8].unsqueeze(2).to_broadcast([128, 8, 64]), op=ALU.mult),
                        r=("ps7",), w=(f"ytmp{p}_{g}",))
                R.op("dve", lambda e: e.tensor_tensor(out=v3(H), in0=v3(H), in1=cdec[:, c, d * 16:(d + 1) * 16].unsqueeze(2).to_broadcast([128, 16, 64]),
                                                      op=ALU.mult), r=(f"Hin{d}", "Hb"), w=(f"Hin{d}",))
                for g in range(2):
                    R.op("pe", lambda e, g=g: e.matmul(ps[7][:, :], lhsT=X_tm[:, c, 1024 + g * 128:1024 + (g + 1) * 128],
                                                       rhs=xw2[p][:, g * 512:(g + 1) * 512], start=True, stop=True),
                         r=(f"xw2{p}",), w=("ps7",))
                    R.op("dve", lambda e, g=g: e.tensor_tensor(out=H[:, g * 512:(g + 1) * 512], in0=H[:, g * 512:(g + 1) * 512],
                                                              in1=ps[7][:, :], op=ALU.add), r=(f"Hin{d}", "ps7"), w=(f"Hin{d}",))
                R.op("act", lambda e: e.activation(out=Hb, in_=H, func=AF.Copy), r=(f"Hin{d}",), w=("Hb",))
                for g in range(2):
                    gs = slice(g * 512, (g + 1) * 512)
                    if d == 0:
                        R.op("dve", lambda e, g=g, gs=gs: e.tensor_tensor(out=yacc[:, c, gs], in0=ps[5 + g][:, :], in1=ytmp[p][:, gs], op=ALU.add),
                             r=(f"ps{5 + g}", f"ytmp{p}_{g}"), w=(f"yacc{c}_{g}",))
                    else:
                        R.op("dve", lambda e, g=g, gs=gs: e.tensor_tensor(out=ytot[p][:, gs], in0=ps[5 + g][:, :], in1=ytmp[p][:, gs], op=ALU.add),
                             r=(f"ps{5 + g}", f"ytmp{p}_{g}"), w=(f"ytot{p}_{g}",))
                        R.op("pool", lambda e, gs=gs: e.tensor_tensor(out=ytot[p][:, gs], in0=ytot[p][:, gs], in1=yacc[:, c, gs], op=ALU.add),
                             r=(f"ytot{p}_{g}", f"yacc{c}_{g}"), w=(f"ytot{p}_{g}",))
                        R.op("pool", lambda e, g=g, gs=gs: e.tensor_tensor(
                            out=v3(ytmp[p][:, gs]), in0=v3(X_tm[:, c, gs]),
                            in1=dskip[:, g * 8:(g + 1) * 8].unsqueeze(2).to_broadcast([128, 8, 64]), op=ALU.mult),
                            r=(f"ytot{p}_{g}",), w=(f"ytmp{p}_{g}",))
                        R.op("pool", lambda e, gs=gs: e.tensor_tensor(out=ytot[p][:, gs], in0=ytot[p][:, gs], in1=ytmp[p][:, gs], op=ALU.add),
                             r=(f"ytmp{p}_{g}", f"ytot{p}_{g}"), w=(f"ytot{p}_{g}",))
                        R.op("pool", lambda e, gs=gs: e.tensor_tensor(out=ytot[p][:, gs], in0=ytot[p][:, gs], in1=zsb[p][:, gs], op=ALU.mult),
                             r=(f"ytot{p}_{g}", f"zsb{p}"), w=(f"ytot{p}_{g}",))
                if d == 1:
                    yk = (f"ytot{p}_0", f"ytot{p}_1")
                    R.op("act", lambda e: e.activation(out=gsq, in_=ytot[p], func=AF.Square, accum_out=ssg[:, c:c + 1]),
                         r=yk, w=("gsq", f"ssg{c}"))
                    R.op("dve", lambda e: e.tensor_scalar(out=tg[:, c:c + 1], in0=ssg[:, c:c + 1], scalar1=1.0 / 1024, scalar2=EPS,
                                                          op0=ALU.mult, op1=ALU.add), r=(f"ssg{c}",), w=(f"tg{c}",))
                    R.op("act", lambda e: e.activation(out=sg_[:, c:c + 1], in_=tg[:, c:c + 1], func=AF.Sqrt), r=(f"tg{c}",), w=(f"sg{c}",))
                    R.op("dve", lambda e: e.reciprocal(out=rg[:, c:c + 1], in_=sg_[:, c:c + 1]), r=(f"sg{c}",), w=(f"rg{c}",))
                    R.op("dve", lambda e: e.scalar_tensor_tensor(out=yacc[:, c, :], in0=ytot[p], scalar=rg[:, c:c + 1], in1=gssd,
                                                                 op0=ALU.mult, op1=ALU.mult),
                         r=yk + (f"rg{c}", "gssd", f"yacc{c}_0", f"yacc{c}_1"), w=(f"yn{c}",))

            front(0)
            for n_i in range(16):
                if n_i + 1 < 16:
                    front(n_i + 1)
                tail(n_i)
            if d == 0:
                dump("yF0", yacc[:, 0, :], [128, 1024], ("yacc0_0", "yacc0_1"), BF16)
                dump("yF5", yacc[:, 5, :], [128, 1024], ("yacc5_0", "yacc5_1"), BF16)
        dump("yn3", yacc[:, 3, :], [128, 1024], ("yn3",), BF16)
        dump("yn12", yacc[:, 12, :], [128, 1024], ("yn12",), BF16)
        R.flush()
    top["B"] = baseB
    if stop_after <= 4:
        return finish()

    h2T = sb("h2T", [128, 8, 2048], BF16)
    combT = sb("combT", [32, 2048])
    top["A"] = 8192
    with Mark(), Mark("A"):
        Wout = sb("Wout", [128, 16, 1024], BF16, ar="A")
        Wr = sb("Wr", [128, 8, 32])
        brt = sb("brt", [128, 32])
        catT = [sb(f"catT{i}", [128, 16, 128], BF16) for i in range(2)]
        xt = [sb(f"xt5_{i}", [128, 1024]) for i in range(2)]
        at = [sb(f"at5_{i}", [128, 1024], BF16) for i in range(2)]
        r1 = [sb(f"r1_{i}", [128, 1024]) for i in range(2)]
        hm2 = [sb(f"hm2_{i}", [128, 1024]) for i in range(2)]
        h32 = [sb(f"h32_{i}", [128, 8, 128]) for i in range(2)]
        junk = sb("junk5", [128, 1024], BF16)
        sv = sb("sv5", [128, 16, 8])
        lg = [sb(f"lg{i}", [128, 32]) for i in range(2)]
        mx8 = [sb(f"mx8_{i}", [128, 8]) for i in range(2)]
        msk = [sb(f"msk{i}", [128, 32]) for i in range(2)]
        exr = [sb(f"exr{i}", [128, 32]) for i in range(2)]
        cmb = [sb(f"cmb{i}", [128, 32]) for i in range(2)]
        R.dma("pool", lambda e: e.dma_start(out=Wout, in_=w_out), w=("Wout",), sem="d_w2")
        R.dma("sp", lambda e: e.dma_start(out=Wr, in_=w_router), w=("Wr",), sem="d_c")
        R.dma("sp", lambda e: e.dma_start(out=brt, in_=b_router_row.partition_broadcast(128)), w=("brt",), sem="d_c")
        pb0 = ps[0][:, :].bitcast(BF16)
        pb1 = ps[1][:, :].bitcast(BF16)
        def p5_stageA(i):
            b = i % 2
            R.dma("sp", lambda e, i=i, b=b: e.dma_start(out=xt[b], in_=xh[(8 + i) * 128:(9 + i) * 128, :]), w=(f"xt{b}",), sem=f"d_xt{b}")
            R.dma("sp", lambda e, i=i, b=b: e.dma_start(out=at[b], in_=attn_d[i * 128:(i + 1) * 128, :]), r=("attn_d",), w=(f"at{b}",), sem=f"d_at{b}")

            def tra(e, b=b):
                ins = None
                for f in range(8):
                    ins = e.transpose(out=pb0[:, f * 128:(f + 1) * 128], in_=at[b][:, f * 128:(f + 1) * 128], identity=identb[:])
                return ins
            R.op("pe", tra, r=(f"at{b}", "identb"), w=("ps0",))
            R.op("act", lambda e, b=b: e.activation(out=catT[b][:, 0:8, :].rearrange("p f t -> p (f t)"), in_=pb0[:, 0:1024], func=AF.Copy),
                 r=("ps0",), w=(f"catT{b}a",))

            def try_(e, i=i):
                ins = None
                for f in range(8):
                    ins = e.transpose(out=pb1[:, f * 128:(f + 1) * 128], in_=yacc[:, i, f * 128:(f + 1) * 128], identity=identb[:])
                return ins
            R.op("pe", try_, w=("ps1",))
            R.op("dve", lambda e, b=b: e.tensor_copy(out=catT[b][:, 8:16, :].rearrange("p f t -> p (f t)"), in_=pb1[:, 0:1024]),
                 r=("ps1",), w=(f"catT{b}b",))

            def mmo(e, b=b):
                ins = None
                for hf in range(2):
                    for k in range(16):
                        ins = e.matmul(ps[2 + hf][:, :], lhsT=catT[b][:, k, :], rhs=Wout[:, k, hf * 512:(hf + 1) * 512],
                                       start=(k == 0), stop=(k == 15))
                return ins
            R.op("pe", mmo, r=(f"catT{b}a", f"catT{b}b", "Wout"), w=("ps2", "ps3"))
            for hf in range(2):
                R.op("act", lambda e, i=i, hf=hf: e.activation(out=junk[:, hf * 512:(hf + 1) * 512], in_=ps[2 + hf][:, :], func=AF.Square,
                                                               accum_out=sv[:, i, hf:hf + 1]), r=(f"ps{2 + hf}",), w=(f"junk{hf}", f"sv{i}_{hf}"))
            R.op("dve", lambda e, i=i: e.tensor_tensor(out=sv[:, i, 2:3], in0=sv[:, i, 0:1], in1=sv[:, i, 1:2], op=ALU.add),
                 r=(f"sv{i}_0", f"sv{i}_1"), w=(f"sv{i}_2",))
            R.op("dve", lambda e, i=i: e.tensor_scalar(out=sv[:, i, 3:4], in0=sv[:, i, 2:3], scalar1=1.0 / 1024, scalar2=EPS, op0=ALU.mult,
                                                       op1=ALU.add), r=(f"sv{i}_2",), w=(f"sv{i}_3",))
            R.op("act", lambda e, i=i: e.activation(out=sv[:, i, 4:5], in_=sv[:, i, 3:4], func=AF.Sqrt), r=(f"sv{i}_3",), w=(f"sv{i}_4",))
            R.op("dve", lambda e, i=i: e.reciprocal(out=sv[:, i, 5:6], in_=sv[:, i, 4:5]), r=(f"sv{i}_4",), w=(f"sv{i}_5",))
            for hf in range(2):
                hsl = slice(hf * 512, (hf + 1) * 512)
                R.op("dve", lambda e, i=i, b=b, hf=hf, hsl=hsl: e.scalar_tensor_tensor(out=r1[b][:, hsl], in0=ps[2 + hf][:, :], scalar=sv[:, i, 5:6],
                                                                                      in1=Gm[:, hsl], op0=ALU.mult, op1=ALU.mult),
                     r=(f"ps{2 + hf}", f"sv{i}_5", f"junk{hf}"), w=(f"r1_{b}_{hf}",))
                R.op("pool", lambda e, b=b, hsl=hsl: e.tensor_tensor(out=r1[b][:, hsl], in0=r1[b][:, hsl], in1=xt[b][:, hsl], op=ALU.add),
                     r=(f"r1_{b}_{hf}", f"xt{b}"), w=(f"r1_{b}_{hf}",))
            rk = (f"r1_{b}_0", f"r1_{b}_1")
            R.dma("sp", lambda e, i=i, b=b: e.dma_start(out=res1_d[i * 128:(i + 1) * 128, :], in_=r1[b]), r=rk, w=(f"res1d{i}",), sem=f"d_r1{b}")
            if i == 2:
                dump("res1_2", r1[b], [128, 1024], rk)
            R.op("act", lambda e, i=i, b=b: e.activation(out=junk, in_=r1[b], func=AF.Square, accum_out=sv[:, i, 6:7]),
                 r=rk, w=("junk0", "junk1", f"sv{i}_6"))
            R.op("dve", lambda e, i=i: e.tensor_scalar(out=sv[:, i, 7:8], in0=sv[:, i, 6:7], scalar1=1.0 / 1024, scalar2=EPS, op0=ALU.mult,
                                                       op1=ALU.add), r=(f"sv{i}_6",), w=(f"sv{i}_7",))
            R.op("act", lambda e, i=i: e.activation(out=sv[:, i, 6:7], in_=sv[:, i, 7:8], func=AF.Sqrt), r=(f"sv{i}_7",), w=(f"sv{i}_6",))
            R.op("dve", lambda e, i=i: e.reciprocal(out=sv[:, i, 7:8], in_=sv[:, i, 6:7]), r=(f"sv{i}_6",), w=(f"sv{i}_7",))
            R.op("dve", lambda e, i=i, b=b: e.scalar_tensor_tensor(out=hm2[b], in0=r1[b], scalar=sv[:, i, 7:8], in1=Arow_f, op0=ALU.mult, op1=ALU.mult),
                 r=rk + (f"sv{i}_7",), w=(f"hm2{b}",))
            R.op("pool", lambda e, b=b: e.tensor_tensor(out=hm2[b], in0=hm2[b], in1=Brow_f, op=ALU.add), r=(f"hm2{b}",), w=(f"hm2{b}",))

            def mmt(e, b=b):
                ins = None
                for k in range(8):
                    ins = e.matmul(ps[4 + k // 4][:, (k % 4) * 128:(k % 4 + 1) * 128], lhsT=hm2[b][:, k * 128:(k + 1) * 128], rhs=ident[:],
                                   start=True, stop=True)
                return ins
            R.op("pe", mmt, r=(f"hm2{b}",), w=("ps4", "ps5"))
            p4v = ps[4][:, :].rearrange("p (k t) -> p k t", k=4)
            p5v = ps[5][:, :].rearrange("p (k t) -> p k t", k=4)
            R.op("act", lambda e, i=i, p4v=p4v: e.activation(out=h2T[:, 0:4, i * 128:(i + 1) * 128], in_=p4v, func=AF.Copy), r=("ps4",), w=(f"h2T{i}a",))
            R.op("act", lambda e, b=b, p4v=p4v: e.activation(out=h32[b][:, 0:4, :], in_=p4v, func=AF.Copy), r=("ps4",), w=(f"h32_{b}a",))
            R.op("dve", lambda e, i=i, p5v=p5v: e.tensor_copy(out=h2T[:, 4:8, i * 128:(i + 1) * 128], in_=p5v), r=("ps5",), w=(f"h2T{i}b",))
            R.op("dve", lambda e, b=b, p5v=p5v: e.tensor_copy(out=h32[b][:, 4:8, :], in_=p5v), r=("ps5",), w=(f"h32_{b}b",))

        def p5_stageB(i):
            b = i % 2
            def mmr(e, b=b):
                ins = None
                for k in range(8):
                    ins = e.matmul(ps[6][:, 0:32], lhsT=h32[b][:, k, :], rhs=Wr[:, k, :], start=(k == 0), stop=(k == 7))
                return ins
            R.op("pe", mmr, r=(f"h32_{b}a", f"h32_{b}b", "Wr"), w=("ps6",))
            R.op("dve", lambda e, b=b: e.tensor_tensor(out=lg[b], in0=ps[6][:, 0:32], in1=brt, op=ALU.add), r=("ps6", "brt"), w=(f"lg{b}",))
            R.op("dve", lambda e, b=b: e.max(out=mx8[b], in_=lg[b]), r=(f"lg{b}",), w=(f"mx8{b}",))
            R.op("dve", lambda e, b=b: e.tensor_scalar(out=msk[b], in0=lg[b], scalar1=mx8[b][:, 3:4], scalar2=None, op0=ALU.is_ge),
                 r=(f"lg{b}", f"mx8{b}"), w=(f"msk{b}",))
            R.op("dve", lambda e, b=b: e.tensor_scalar(out=exr[b], in0=lg[b], scalar1=mx8[b][:, 0:1], scalar2=None, op0=ALU.subtract),
                 r=(f"lg{b}", f"mx8{b}"), w=(f"exr{b}",))
            R.op("act", lambda e, b=b: e.activation(out=exr[b], in_=exr[b], func=AF.Exp), r=(f"exr{b}",), w=(f"exr{b}",))
            R.op("dve", lambda e, b=b: e.tensor_tensor(out=exr[b], in0=exr[b], in1=msk[b], op=ALU.mult), r=(f"exr{b}", f"msk{b}"), w=(f"exr{b}",))
            R.op("dve", lambda e, b=b: e.reduce_sum(out=mx8[b][:, 4:5], in_=exr[b], axis=AX.X), r=(f"exr{b}",), w=(f"mx8{b}",))
            R.op("dve", lambda e, b=b: e.reciprocal(out=mx8[b][:, 5:6], in_=mx8[b][:, 4:5]), r=(f"mx8{b}",), w=(f"mx8{b}",))
            R.op("dve", lambda e, b=b: e.tensor_scalar(out=cmb[b], in0=exr[b], scalar1=mx8[b][:, 5:6], scalar2=None, op0=ALU.mult),
                 r=(f"exr{b}", f"mx8{b}"), w=(f"cmb{b}",))
            R.op("pe", lambda e, b=b: e.transpose(out=ps[7][0:32, 0:128], in_=cmb[b], identity=ident[:]), r=(f"cmb{b}",), w=("ps7",))
            R.op("act", lambda e, i=i: e.activation(out=combT[:, i * 128:(i + 1) * 128], in_=ps[7][0:32, 0:128], func=AF.Copy),
                 r=("ps7",), w=(f"combT{i}",))
            if i == 2:
                dump("cmb2", cmb[b], [128, 32], (f"cmb{b}",))
                dump("lg2", lg[b], [128, 32], (f"lg{b}",))

        p5_stageA(0)
        for i in range(16):
            if i + 1 < 16:
                p5_stageA(i + 1)
            p5_stageB(i)
        dump("h2T", h2T[:, :, 256:384], [128, 8, 128], ("h2T2a", "h2T2b"), BF16)
        R.dma("sp", lambda e: e.dma_start(out=comb_d, in_=combT), r=tuple(f"combT{i}" for i in range(16)), w=("comb_d",), sem="d_comb")
        R.flush()
    if stop_after <= 5:
        return finish()

    top["A"] = 0
    with Mark(), Mark("A"):
        oacc = sb("oacc", [128, 8, 1024], ar="A")
        Dw = [sb(f"Dw{i}", [128, 8, 1024], BF16, ar="A") for i in range(2)]
        GUr = [sb(f"GUr{i}", [128, 8, 512], BF16) for i in range(3)]
        actT = sb("actT", [128, 8, 1024], BF16)
        cbc = [sb(f"cbc{i}", [128, 1024]) for i in range(2)]
        bguF = sb("bguF", [128, 512])
        bguS = sb("bguS", [128, 512])
        bdn = sb("bdn", [32, 1024])
        gp = [sb(f"gp{i}", [128, 512]) for i in range(2)]
        sg = [sb(f"sg{i}", [128, 512], BF16) for i in range(2)]
        ub = [sb(f"ub{i}", [128, 512]) for i in range(2)]
        uc = [sb(f"uc{i}", [128, 512]) for i in range(2)]
        t1 = [sb(f"t1_{i}", [128, 512], BF16) for i in range(2)]
        t2 = [sb(f"t2_{i}", [128, 512], BF16) for i in range(2)]
        fsv = sb("fsv", [128, 16, 8])
        fjunk = sb("fjunk", [128, 512], BF16)
        fin32 = actT.rearrange("p k f -> p (k f)").bitcast(F32)
        r1f = [fin32[:, 0:1024], fin32[:, 1024:2048]]
        outf = [fin32[:, 2048:3072], fin32[:, 3072:4096]]
        AK = ("actT0", "actT1")
        R.dma("sp", lambda e: e.dma_start(out=bguF, in_=bguF_d), w=("bguF",), sem="d_c")
        R.dma("sp", lambda e: e.dma_start(out=bdn, in_=b_down_d), w=("bdn",), sem="d_c")
        R.op("dve", lambda e: e.tensor_scalar(out=bguS, in0=bguF, scalar1=1.702, scalar2=None, op0=ALU.mult), r=("bguF",), w=("bguS",))

        NP = 2 * NE * 4

        def issue_piece(n):
            if n >= NP:
                return
            ex = (n // 4) % NE
            q4 = n % 4
            slot = n % 3
            wv = w_gu[ex].rearrange("(k p) f -> p k f", p=128)
            R.dma("pool", lambda e: e.dma_start(out=GUr[slot][:, :, 0:256], in_=wv[:, :, q4 * 256:(q4 + 1) * 256]),
                  w=(f"GUr{slot}",), sem=f"d_GU{slot}")
            R.dma("pool", lambda e: e.dma_start(out=GUr[slot][:, :, 256:512], in_=wv[:, :, 1024 + q4 * 256:1024 + (q4 + 1) * 256]),
                  w=(f"GUr{slot}",), sem=f"d_GU{slot}")

        def issue_dw(m):
            if m >= 2 * NE:
                return
            ex = m % NE
            wv = w_dn[ex].rearrange("(k p) f -> p k f", p=128)
            for q2 in range(2):
                R.dma("pool", lambda e, q2=q2: e.dma_start(out=Dw[m % 2][:, :, q2 * 512:(q2 + 1) * 512], in_=wv[:, :, q2 * 512:(q2 + 1) * 512]),
                      w=(f"Dw{m % 2}",), sem=f"d_Dw{m % 2}")

        def issue_cbc(m):
            if m >= 2 * NE:
                return
            ex = m % NE
            t0_ = (m // NE) * 1024
            R.dma("sp", lambda e: e.dma_start(out=cbc[m % 2], in_=comb_d[ex, t0_:t0_ + 1024].partition_broadcast(128)),
                  r=("comb_d",), w=(f"cbc{m % 2}",), sem=f"d_cbc{m % 2}")

        issue_piece(0)
        issue_piece(1)
        issue_dw(0)
        issue_cbc(0)
        it = 0
        dit = 0
        for half in range(2):
            t0 = half * 1024
            for tb in range(2):
                for dc in range(8):
                    pk = 4 + dc % 2
                    R.op("pe", lambda e, tb=tb, dc=dc, pk=pk, t0=t0: e.matmul(ps[pk][:, :], lhsT=bdn[:, dc * 128:(dc + 1) * 128],
                                                                              rhs=combT[:, t0 + tb * 512:t0 + (tb + 1) * 512], start=True, stop=True),
                         r=("bdn",), w=(f"ps{pk}",))
                    R.op("act", lambda e, tb=tb, dc=dc, pk=pk: e.activation(out=oacc[:, dc, tb * 512:(tb + 1) * 512], in_=ps[pk][:, :], func=AF.Copy),
                         r=(f"ps{pk}",), w=(f"oacc{dc}_{tb}",))
            for ex in range(NE):
                m = half * NE + ex
                issue_dw(m + 1)
                issue_cbc(m + 1)
                cb = m % 2
                for q4 in range(4):
                    n = m * 4 + q4
                    slot = n % 3
                    issue_piece(n + 2)
                    for tb in range(2):
                        tsl = slice(t0 + tb * 512, t0 + (tb + 1) * 512)
                        for fcl in range(2):
                            fc = q4 * 2 + fcl
                            p2 = it % 2
                            it += 1
                            pg, pu = 2 * p2, 2 * p2 + 1

                            def mmg(e, slot=slot, fcl=fcl, tsl=tsl, pg=pg, pu=pu):
                                ins = None
                                for k in range(8):
                                    ins = e.matmul(ps[pg][:, :], lhsT=GUr[slot][:, k, fcl * 128:(fcl + 1) * 128], rhs=h2T[:, k, tsl],
                                                   start=(k == 0), stop=(k == 7))
                                for k in range(8):
                                    ins = e.matmul(ps[pu][:, :], lhsT=GUr[slot][:, k, 256 + fcl * 128:256 + (fcl + 1) * 128], rhs=h2T[:, k, tsl],
                                                   start=(k == 0), stop=(k == 7))
                                return ins
                            R.op("pe", mmg, r=(f"GUr{slot}",), w=(f"ps{pg}", f"ps{pu}"))
                            bi = ex * 16 + fc
                            R.op("act", lambda e, p2=p2, pg=pg, bi=bi: e.activation(out=sg[p2], in_=ps[pg][:, :], func=AF.Sigmoid,
                                                                                   bias=bguS[:, bi:bi + 1], scale=1.702),
                                 r=(f"ps{pg}", "bguS"), w=(f"sg{p2}",))
                            R.op("act", lambda e, p2=p2, pu=pu, bi=bi: e.activation(out=ub[p2], in_=ps[pu][:, :], func=AF.Identity,
                                                                                   bias=bguF[:, bi + 8:bi + 9]),
                                 r=(f"ps{pu}", "bguF"), w=(f"ub{p2}",))
                            R.op("dve", lambda e, p2=p2, pg=pg, bi=bi: e.tensor_scalar(out=gp[p2], in0=ps[pg][:, :], scalar1=bguF[:, bi:bi + 1],
                                                                                      scalar2=7.0, op0=ALU.add, op1=ALU.min),
                                 r=(f"ps{pg}", "bguF", f"sg{p2}"), w=(f"gp{p2}",))
                            R.op("dve", lambda e, p2=p2: e.tensor_scalar(out=uc[p2], in0=ub[p2], scalar1=-7.0, scalar2=7.0, op0=ALU.max, op1=ALU.min),
                                 r=(f"ub{p2}",), w=(f"uc{p2}",))
                            R.op("dve", lambda e, p2=p2: e.tensor_tensor(out=t1[p2], in0=gp[p2], in1=sg[p2], op=ALU.mult),
                                 r=(f"gp{p2}", f"sg{p2}"), w=(f"t1_{p2}",))
                            R.op("dve", lambda e, p2=p2: e.scalar_tensor_tensor(out=t2[p2], in0=uc[p2], scalar=1.0, in1=t1[p2], op0=ALU.add, op1=ALU.mult),
                                 r=(f"uc{p2}", f"t1_{p2}"), w=(f"t2_{p2}",))
                            R.op("dve", lambda e, p2=p2, fc=fc, cb=cb, tb=tb: e.tensor_tensor(
                                out=actT[:, fc, tb * 512:(tb + 1) * 512], in0=t2[p2], in1=cbc[cb][:, tb * 512:(tb + 1) * 512], op=ALU.mult),
                                r=(f"t2_{p2}", f"cbc{cb}"), w=(f"actT{tb}",))
                for tb in range(2):
                    for dc in range(8):
                        pk = 4 + dit % 2
                        dit += 1

                        def mmd(e, m=m, dc=dc, pk=pk, tb=tb):
                            ins = None
                            for k in range(8):
                                ins = e.matmul(ps[pk][:, :], lhsT=Dw[m % 2][:, k, dc * 128:(dc + 1) * 128], rhs=actT[:, k, tb * 512:(tb + 1) * 512],
                                               start=(k == 0), stop=(k == 7))
                            return ins
                        R.op("pe", mmd, r=(f"Dw{m % 2}", f"actT{tb}"), w=(f"ps{pk}",))
                        R.op("dve", lambda e, dc=dc, tb=tb, pk=pk: e.tensor_tensor(out=oacc[:, dc, tb * 512:(tb + 1) * 512],
                                                                                  in0=oacc[:, dc, tb * 512:(tb + 1) * 512], in1=ps[pk][:, :], op=ALU.add),
                             r=(f"ps{pk}", f"oacc{dc}_{tb}"), w=(f"oacc{dc}_{tb}",))
            for ti in range(8):
                i = half * 8 + ti
                b = ti % 2
                R.dma("sp", lambda e, i=i, b=b: e.dma_start(out=r1f[b], in_=res1_d[i * 128:(i + 1) * 128, :]), r=(f"res1d{i}",),
                      w=(f"r1f{b}",) + AK, sem=f"d_r1f{b}")

                def trf(e, ti=ti):
                    ins = None
                    for dc in range(8):
                        ins = e.transpose(out=ps[6 + dc // 4][:, (dc % 4) * 128:(dc % 4 + 1) * 128], in_=oacc[:, dc, ti * 128:(ti + 1) * 128],
                                          identity=ident[:])
                    return ins
                R.op("pe", trf, r=tuple(f"oacc{dc}_{ti // 4}" for dc in range(8)), w=("ps6", "ps7"))
                for hf in range(2):
                    R.op("act", lambda e, i=i, hf=hf: e.activation(out=fjunk, in_=ps[6 + hf][:, :], func=AF.Square, accum_out=fsv[:, i, hf:hf + 1]),
                         r=(f"ps{6 + hf}",), w=("fjunk", f"fsv{i}_{hf}"))
                R.op("dve", lambda e, i=i: e.tensor_tensor(out=fsv[:, i, 2:3], in0=fsv[:, i, 0:1], in1=fsv[:, i, 1:2], op=ALU.add),
                     r=(f"fsv{i}_0", f"fsv{i}_1"), w=(f"fsv{i}_2",))
                R.op("dve", lambda e, i=i: e.tensor_scalar(out=fsv[:, i, 3:4], in0=fsv[:, i, 2:3], scalar1=1.0 / 1024, scalar2=EPS, op0=ALU.mult,
                                                           op1=ALU.add), r=(f"fsv{i}_2",), w=(f"fsv{i}_3",))
                R.op("act", lambda e, i=i: e.activation(out=fsv[:, i, 4:5], in_=fsv[:, i, 3:4], func=AF.Sqrt), r=(f"fsv{i}_3",), w=(f"fsv{i}_4",))
                R.op("dve", lambda e, i=i: e.reciprocal(out=fsv[:, i, 5:6], in_=fsv[:, i, 4:5]), r=(f"fsv{i}_4",), w=(f"fsv{i}_5",))
                for hf in range(2):
                    hsl = slice(hf * 512, (hf + 1) * 512)
                    R.op("dve", lambda e, i=i, b=b, hf=hf, hsl=hsl: e.scalar_tensor_tensor(out=outf[b][:, hsl], in0=ps[6 + hf][:, :], scalar=fsv[:, i, 5:6],
                                                                                          in1=Gf[:, hsl], op0=ALU.mult, op1=ALU.mult),
                         r=(f"ps{6 + hf}", f"fsv{i}_5"), w=(f"outf{b}_{hf}",) + AK)
                    R.op("dve", lambda e, b=b, hsl=hsl: e.tensor_tensor(out=outf[b][:, hsl], in0=outf[b][:, hsl], in1=r1f[b][:, hsl], op=ALU.add),
                         r=(f"outf{b}_{hf}", f"r1f{b}"), w=(f"outf{b}_{hf}",))
                R.dma("sp", lambda e, i=i, b=b: e.dma_start(out=out_d[i * 128:(i + 1) * 128, :], in_=outf[b]),
                      r=(f"outf{b}_0", f"outf{b}_1") + AK, w=(f"outd{i}",), sem="d_out")
        R.flush()
    return finish()


def _consts():
    idx = np.arange(128)
    rr, cc = idx[:, None], idx[None, :]
    masks = np.concatenate([(rr <= cc), (rr >= cc), (rr > cc), (rr < cc), np.ones((128, 128), bool)], axis=1).astype(np.float32)
    slopes = 2.0 ** (-8.0 * np.arange(1, 17) / 16.0)
    o = np.arange(17)
    delta = (o[None, :, None] - 8) * 128 + idx[:, None, None] - idx[None, None, :]
    ad = np.abs(delta)
    mult = (ad <= 64).astype(np.float64) + ((delta % 4 == 0) & (ad <= 256)) + ((delta % 16 == 0) & (ad <= 1024))
    etab = np.stack([mult * np.exp(-s * ad) for s in slopes]).astype(np.float32).reshape(16, 128, 17 * 128)
    sele = np.zeros((32, 32, 128), np.float32)
    for e in range(32):
        sele[e, e, :] = 1.0
    return masks, etab, sele.reshape(32, 32 * 128)


def make_in_maps(inp):
    f = lambda a: np.ascontiguousarray(np.asarray(a, dtype=np.float32))
    x = f(inp["x"])
    masks, etab, sele = _consts()
    common = {
        "w_ada": f(inp["w_ada"][0]), "b_ada_row": f(inp["b_ada"][0]),
        "g_pre_mix": f(inp["g_pre_mix"][0]), "g_pre_ffn": f(inp["g_pre_ffn"][0]),
        "g_post_mix": f(inp["g_post_mix"][0]), "g_post_ffn": f(inp["g_post_ffn"][0]), "g_ssd_norm": f(inp["g_ssd_norm"][0]),
        "w_in": f(inp["w_in"][0]),
        "convw": f(inp["conv_w"][0].reshape(5, 12, 128).transpose(2, 1, 0).reshape(128, 60)),
        "convb": f(inp["conv_b"][0].reshape(12, 128).T),
        "dt_bias": f(inp["dt_bias"][0].reshape(32)), "a_log": f(inp["a_log"][0].reshape(32)), "d_skip": f(inp["d_skip"][0]),
        "w_out": f(inp["w_out"][0]), "w_router": f(inp["w_router"][0]), "b_router": f(inp["b_router"][0]),
        "w_gate_up": f(inp["w_gate_up"][0]), "w_down": f(inp["w_down"][0]),
        "bguF": f(inp["b_gate_up"][0].reshape(32, 16, 128).transpose(2, 0, 1).reshape(128, 512)),
        "b_down": f(inp["b_down"][0]),
        "ident": np.eye(128, dtype=np.float32), "masks": masks, "etab": etab,
    }
    maps = []
    for r in range(NCORES):
        b, j = r // 4, r % 4
        lo = j * TOK - 1024
        xhp = np.zeros((4096, 1024), np.float32)
        s0, s1 = max(lo, 0), min(lo + 4096, 8192)
        xhp[s0 - lo:s1 - lo] = x[b, s0:s1]
        tokidx = lo + np.arange(4096)
        val = ((tokidx >= 0) & (tokidx < 8192)).astype(np.float32).reshape(32, 128).T
        edge = np.zeros((128, 2), np.float32)
        edge[:, 0] = 1.0 if j > 0 else 0.0
        edge[:, 1] = 1.0 if j < 3 else 0.0
        selv = np.zeros((128, 4), np.float32)
        selv[:, j] = 1.0
        m = dict(common)
        m.update({"xh": xhp, "valid": np.ascontiguousarray(val), "edge": edge, "sel": selv,
                  "cvec": f(np.asarray(inp["c"])[b].reshape(8, 128).T)})
        maps.append(m)
    return maps


def kernel(**inputs):
    nc, es, _ = build()
    with es:
        pass
    in_maps = make_in_maps(inputs)
    res = run_bass_kernel_spmd(nc, in_maps, core_ids=list(range(NCORES)))
    out = np.zeros((2, 8192, 1024), np.float32)
    for r in range(NCORES):
        b, j = r // 4, r % 4
        out[b, j * TOK:(j + 1) * TOK] = res.results[r]["out"]
    return out
```

```python
from contextlib import ExitStack
import numpy as np
import concourse.bass as bass
import concourse.mybir as mybir
from concourse.bass_utils import run_bass_kernel_spmd

F32 = mybir.dt.float32
BF16 = mybir.dt.bfloat16
AF = mybir.ActivationFunctionType
ALU = mybir.AluOpType
AX = mybir.AxisListType

NCORES = 8
TOK = 2048
NT = 16
NTH = 32
EPS = 1e-6
NE = 32


class Rec:
    def __init__(self, nc, es):
        self.nc = nc
        self.es = es
        self.q = {e: [] for e in ("pe", "act", "dve", "pool", "sp")}
        self.semh = {}
        self.cnt = {}
        self.waited = {e: {} for e in self.q}
        self.lw = {}
        self.rd = {}

    def sem(self, key):
        if key not in self.semh:
            self.semh[key] = self.es.enter_context(self.nc.semaphore("s_" + key))
            self.cnt[key] = 0
        return self.semh[key]

    def op(self, eng, fn, r=(), w=(), semkey=None, inc=1):
        semkey = semkey or eng
        self.sem(semkey)
        deps = []
        for k in r:
            if k in self.lw:
                deps.append(self.lw[k])
        for k in w:
            if k in self.lw:
                deps.append(self.lw[k])
            deps += self.rd.get(k, [])
        need = {}
        for (sk, v) in deps:
            if sk.startswith("d_"):
                v = max(v, self.cnt[sk])
            if v > self.waited[eng].get(sk, 0):
                need[sk] = max(need.get(sk, 0), v)
        for sk, v in need.items():
            self.waited[eng][sk] = v
        self.cnt[semkey] += (inc if inc else 1)
        tok = (semkey, self.cnt[semkey])
        self.q[eng].append((list(need.items()), fn, semkey, inc))
        for k in r:
            self.rd.setdefault(k, []).append(tok)
        for k in w:
            self.lw[k] = tok
            self.rd[k] = []
        return tok

    def dma(self, eng, fn, r=(), w=(), sem="d_x"):
        return self.op(eng, fn, r, w, semkey=sem, inc=16)

    def flush(self):
        nc = self.nc
        need = [(sk, v) for sk, v in self.cnt.items() if sk.startswith("d_") and v > self.waited["sp"].get(sk, 0)]
        if need:
            for sk, v in need:
                self.waited["sp"][sk] = v
            self.sem("sp")
            self.cnt["sp"] += 1
            self.q["sp"].append((need, lambda e: e.nop(), "sp", 1))
        q = self.q
        semh = self.semh

        def run(engobj, lst):
            for waits, fn, semkey, inc in lst:
                for sk, v in waits:
                    engobj.wait_ge(semh[sk], v)
                ins = fn(engobj)
                if inc is None:
                    ins.then_inc(semh[semkey])
                else:
                    ins.then_inc(semh[semkey], inc)

        with nc.Block() as block:
            @block.tensor
            def _(e):
                run(e, q["pe"])

            @block.scalar
            def _(e):
                run(e, q["act"])

            @block.vector
            def _(e):
                run(e, q["dve"])

            @block.gpsimd
            def _(e):
                run(e, q["pool"])

            @block.sync
            def _(e):
                run(e, q["sp"])

        self.q = {e: [] for e in q}


def build(dbg=(), stop_after=99):
    nc = bass.Bass("TRN2", target_bir_lowering=False)
    es = ExitStack()
    R = Rec(nc, es)
    dbg_outs = {}

    def din(name, shape, dt=F32):
        return nc.dram_tensor(name, list(shape), dt, kind="ExternalInput").ap()

    CAPA, CAPB = 16384, 35500
    arenaA = es.enter_context(nc.sbuf_tensor("arenaA", [128, CAPA], F32))
    arenaB = es.enter_context(nc.sbuf_tensor("arenaB", [128, CAPB], F32))
    top = {"A": 0, "B": 0}
    cap = {"A": CAPA, "B": CAPB}

    def sb(name, shape, dt=F32, ar="B"):
        shape = list(shape)
        n = int(np.prod(shape[1:]))
        w = n if dt == F32 else (n + 1) // 2
        w = (w + 7) // 8 * 8
        off = top[ar]
        top[ar] += w
        assert top[ar] <= cap[ar], (name, ar, top[ar])
        base = arenaA if ar == "A" else arenaB
        ap = base[0:shape[0], off:off + w]
        if dt != F32:
            ap = ap.bitcast(dt)
        ap = ap[:, 0:n]
        if len(shape) == 3:
            ap = ap.rearrange("p (a b) -> p a b", a=shape[1])
        elif len(shape) == 4:
            ap = ap.rearrange("p (a b c) -> p a b c", a=shape[1], b=shape[2])
        return ap

    class Mark:
        def __init__(self, ar="B"):
            self.ar = ar

        def __enter__(self):
            self.m = top[self.ar]
            return self

        def __exit__(self, *a):
            top[self.ar] = self.m
            return False

    def v3(ap):
        return ap.rearrange("p (h e) -> p h e", e=64)

    xh = din("xh", [4096, 1024])
    valid_d = din("valid", [128, 32])
    edge_d = din("edge", [128, 2])
    sel_d = din("sel", [128, 4])
    cvec_d = din("cvec", [128, 8])
    w_ada = din("w_ada", [1024, 6144]).rearrange("(k p) n -> p k n", p=128)
    b_ada_row = din("b_ada_row", [6144])
    gpre_mix_row = din("g_pre_mix", [1024])
    gpre_ffn_row = din("g_pre_ffn", [1024])
    gpost_mix_row = din("g_post_mix", [1024])
    gpost_ffn_row = din("g_post_ffn", [1024])
    gssd_row = din("g_ssd_norm", [1024])
    w_in = din("w_in", [1024, 5664]).rearrange("(k p) n -> p k n", p=128)
    convw_d = din("convw", [128, 60])
    convb_d = din("convb", [128, 12])
    dtb_row = din("dt_bias", [32])
    alog_row = din("a_log", [32])
    dskip_row = din("d_skip", [16])
    w_out = din("w_out", [2048, 1024]).rearrange("(k p) n -> p k n", p=128)
    w_router = din("w_router", [1024, 32]).rearrange("(k p) n -> p k n", p=128)
    b_router_row = din("b_router", [32])
    w_gu = din("w_gate_up", [32, 1024, 2048]) if stop_after >= 6 else None
    w_dn = din("w_down", [32, 1024, 1024]) if stop_after >= 6 else None
    bguF_d = din("bguF", [128, 512])
    b_down_d = din("b_down", [32, 1024])
    ident_d = din("ident", [128, 128])
    masks_d = din("masks", [128, 5 * 128])
    etab_d = din("etab", [16, 128, 17 * 128])
    out_d = nc.dram_tensor("out", [TOK, 1024], F32, kind="ExternalOutput").ap()
    res1_d = nc.dram_tensor("res1_scr", [TOK, 1024], F32).ap()
    attn_d = nc.dram_tensor("attn_scr", [TOK, 1024], BF16).ap()
    zs_d = nc.dram_tensor("zs_scr", [TOK, 1024], BF16).ap()
    comb_d = nc.dram_tensor("comb_scr", [32, TOK], F32).ap()
    bnc_in = nc.dram_tensor("bnc_in", [128, 2048], F32).ap()
    bnc_out = nc.dram_tensor("bnc_out", [512, 2048], F32).ap()
    bnc2_in = nc.dram_tensor("bnc2_in", [128, 64], F32).ap()
    bnc2_out = nc.dram_tensor("bnc2_out", [512, 64], F32).ap()

    def dump(name, ap_sb, shape, rkeys, dt=F32):
        if name not in dbg:
            return
        d = nc.dram_tensor("dbg_" + name, list(shape), dt, kind="ExternalOutput").ap()
        dbg_outs[name] = d
        R.dma("sp", lambda e: e.dma_start(out=d, in_=ap_sb), r=rkeys, w=("dbgout_" + name,), sem="d_dbg")

    def finish():
        if R.q["sp"] or R.q["pe"] or R.q["dve"] or R.q["act"] or R.q["pool"]:
            R.flush()
        return nc, es, dbg_outs

    ps = [es.enter_context(nc.psum_tensor(f"ps{i}", [128, 512], F32)) for i in range(8)]

    def gsb(name, shape, dt=F32):
        return es.enter_context(nc.sbuf_tensor("g_" + name, list(shape), dt))
    ident = gsb("ident", [128, 128])
    identb = gsb("identb", [128, 128], BF16)
    masks = gsb("masks", [128, 5, 128])
    triLE, triGE, mSU, mSL, ones = (masks[:, i, :] for i in range(5))
    valid = gsb("valid", [128, 32])
    edge = gsb("edge", [128, 8])
    sel = gsb("sel", [128, 8])
    Gf = sb("Gf", [128, 1024])
    Arow_f = sb("Arow_f", [128, 1024])
    Brow_f = sb("Brow_f", [128, 1024])
    Gm = sb("Gm", [128, 1024])
    gssd = sb("gssd", [128, 1024])
    baseB = top["B"]
    Arow_m = sb("Arow_m", [128, 1024])
    Brow_m = sb("Brow_m", [128, 1024])

    R.dma("sp", lambda e: e.dma_start(out=ident[:], in_=ident_d), w=("ident",), sem="d_c")
    R.dma("sp", lambda e: e.dma_start(out=masks[:], in_=masks_d.rearrange("p (a b) -> p a b", a=5)), w=("masks",), sem="d_c")
    R.dma("sp", lambda e: e.dma_start(out=valid[:], in_=valid_d), w=("valid",), sem="d_c")
    R.dma("sp", lambda e: e.dma_start(out=edge[:, 0:2], in_=edge_d), w=("edge",), sem="d_c")
    R.dma("sp", lambda e: e.dma_start(out=sel[:, 0:4], in_=sel_d), w=("sel",), sem="d_c")
    R.dma("sp", lambda e: e.dma_start(out=gssd, in_=gssd_row.partition_broadcast(128)), w=("gssd",), sem="d_c")
    R.op("dve", lambda e: e.tensor_copy(out=identb[:], in_=ident[:]), r=("ident",), w=("identb",))

    with Mark():
        cvec = sb("cvec", [128, 8])
        cs = sb("cs", [128, 8])
        csB = sb("csB", [128, 8, 128])
        wada = [sb(f"wada{i}", [128, 3072]) for i in range(2)]
        brow = sb("brow", [128, 3072])
        g1 = sb("g1", [128, 1024])
        g2 = sb("g2", [128, 1024])
        t1 = sb("t1m", [128, 1024])
        R.dma("sp", lambda e: e.dma_start(out=cvec, in_=cvec_d), w=("cvec",), sem="d_c")
        R.op("act", lambda e: e.activation(out=cs, in_=cvec, func=AF.Silu), r=("cvec",), w=("cs",))
        for k in range(8):
            R.op("dve", lambda e, k=k: e.tensor_copy(out=csB[:, k, :], in_=cs[:, k:k + 1].to_broadcast([128, 128])),
                 r=("cs",), w=(f"csB{k}",))
        for pas in range(2):
            c0 = pas * 3072
            Brow, Arow, Grow = (Brow_m, Arow_m, Gm) if pas == 0 else (Brow_f, Arow_f, Gf)
            R.dma("sp", lambda e, c0=c0: e.dma_start(out=brow, in_=b_ada_row[c0:c0 + 3072].partition_broadcast(128)), w=("brow",), sem="d_c")
            R.dma("sp", lambda e, pas=pas: e.dma_start(out=g1, in_=(gpre_mix_row if pas == 0 else gpre_ffn_row).partition_broadcast(128)),
                  w=("g1",), sem="d_c")
            R.dma("sp", lambda e, pas=pas: e.dma_start(out=g2, in_=(gpost_mix_row if pas == 0 else gpost_ffn_row).partition_broadcast(128)),
                  w=("g2",), sem="d_c")
            for k in range(8):
                wb = wada[k % 2]
                R.dma("sp", lambda e, k=k, wb=wb, c0=c0: e.dma_start(out=wb[:, 0:1536], in_=w_ada[:, k, c0:c0 + 1536]), w=(f"wada{k % 2}",),
                      sem=f"d_wada{k % 2}")
                R.dma("act", lambda e, k=k, wb=wb, c0=c0: e.dma_start(out=wb[:, 1536:3072], in_=w_ada[:, k, c0 + 1536:c0 + 3072]),
                      w=(f"wada{k % 2}",), sem=f"d_wada{k % 2}")

                def mm(e, k=k, wb=wb):
                    ins = None
                    for j in range(6):
                        ins = e.matmul(ps[j][:, :], lhsT=csB[:, k, :], rhs=wb[:, j * 512:(j + 1) * 512], start=(k == 0), stop=(k == 7))
                    return ins
                R.op("pe", mm, r=(f"wada{k % 2}", f"csB{k}"), w=tuple(f"ps{j}" for j in range(6)))
            for j in range(2):
                sl = slice(j * 512, (j + 1) * 512)
                R.op("dve", lambda e, j=j, sl=sl, Brow=Brow: e.tensor_tensor(out=Brow[:, sl], in0=ps[j][:, :], in1=brow[:, sl], op=ALU.add),
                     r=(f"ps{j}", "brow"), w=(f"Brow{pas}",))
                R.op("dve", lambda e, j=j, sl=sl: e.tensor_tensor(out=t1[:, sl], in0=ps[2 + j][:, :], in1=brow[:, 1024 + j * 512:1024 + (j + 1) * 512],
                                                                  op=ALU.add), r=(f"ps{2 + j}", "brow"), w=("t1m",))
                R.op("dve", lambda e, sl=sl, Arow=Arow: e.scalar_tensor_tensor(out=Arow[:, sl], in0=t1[:, sl], scalar=1.0, in1=g1[:, sl],
                                                                              op0=ALU.add, op1=ALU.mult), r=("t1m", "g1"), w=(f"Arow{pas}",))
                R.op("dve", lambda e, j=j, sl=sl: e.tensor_tensor(out=t1[:, sl], in0=ps[4 + j][:, :], in1=brow[:, 2048 + j * 512:2048 + (j + 1) * 512],
                                                                  op=ALU.add), r=(f"ps{4 + j}", "brow", f"Arow{pas}"), w=("t1m",))
                R.op("dve", lambda e, sl=sl, Grow=Grow: e.tensor_tensor(out=Grow[:, sl], in0=t1[:, sl], in1=g2[:, sl], op=ALU.mult),
                     r=("t1m", "g2"), w=(f"Grow{pas}",))
        dump("Arow_m", Arow_m, [128, 1024], ("Arow0",))
        dump("Gf", Gf, [128, 1024], ("Grow1",))
        R.flush()
    if stop_after <= 0:
        return finish()

    hT = sb("hT", [128, 8, 4096], BF16, ar="A")
    with Mark():
        xt = [sb(f"xt{i}", [128, 1024]) for i in range(4)]
        hm = [sb(f"hm{i}", [128, 1024]) for i in range(4)]
        junk = sb("junk", [128, 1024], BF16)
        ss = sb("ss", [128, 32])
        tmpv = sb("tmpv", [128, 32])
        sdv = sb("sdv", [128, 32])
        rstd = sb("rstd", [128, 32])
        def p1_stageA(i):
            b = i % 4
            R.dma("sp" if i % 2 == 0 else "act", lambda e, i=i, b=b: e.dma_start(out=xt[b], in_=xh[i * 128:(i + 1) * 128, :]),
                  w=(f"xt{b}",), sem=f"d_xt{b}")
            R.op("act", lambda e, i=i, b=b: e.activation(out=junk, in_=xt[b], func=AF.Square, accum_out=ss[:, i:i + 1]),
                 r=(f"xt{b}",), w=("junk", f"ss{i}"))
            R.op("dve", lambda e, i=i: e.tensor_scalar(out=tmpv[:, i:i + 1], in0=ss[:, i:i + 1], scalar1=1.0 / 1024, scalar2=EPS,
                                                       op0=ALU.mult, op1=ALU.add), r=(f"ss{i}",), w=(f"tmpv{i}",))
            R.op("act", lambda e, i=i: e.activation(out=sdv[:, i:i + 1], in_=tmpv[:, i:i + 1], func=AF.Sqrt), r=(f"tmpv{i}",), w=(f"sdv{i}",))
            R.op("dve", lambda e, i=i: e.reciprocal(out=rstd[:, i:i + 1], in_=sdv[:, i:i + 1]), r=(f"sdv{i}",), w=(f"rstd{i}",))
            R.op("dve", lambda e, i=i, b=b: e.scalar_tensor_tensor(out=hm[b], in0=xt[b], scalar=rstd[:, i:i + 1], in1=Arow_m,
                                                                   op0=ALU.mult, op1=ALU.mult), r=(f"xt{b}", f"rstd{i}"), w=(f"hm{b}",))
            R.op("dve", lambda e, b=b: e.tensor_tensor(out=hm[b], in0=hm[b], in1=Brow_m, op=ALU.add), r=(f"hm{b}",), w=(f"hm{b}",))
            pb = (i % 4) * 2

            def mm(e, b=b, pb=pb):
                ins = None
                for k in range(8):
                    ins = e.matmul(ps[pb + k // 4][:, (k % 4) * 128:(k % 4 + 1) * 128], lhsT=hm[b][:, k * 128:(k + 1) * 128],
                                   rhs=ident[:], start=True, stop=True)
                return ins
            R.op("pe", mm, r=(f"hm{b}", "ident"), w=(f"ps{pb}", f"ps{pb + 1}"))

        def p1_stageB(i):
            pb = (i % 4) * 2
            R.op("act", lambda e, i=i, pb=pb: e.activation(out=hT[:, 0:4, i * 128:(i + 1) * 128],
                                                           in_=ps[pb][:, :].rearrange("p (k t) -> p k t", k=4), func=AF.Copy),
                 r=(f"ps{pb}",), w=(f"hT{i}a",))
            R.op("dve", lambda e, i=i, pb=pb: e.tensor_copy(out=hT[:, 4:8, i * 128:(i + 1) * 128],
                                                            in_=ps[pb + 1][:, :].rearrange("p (k t) -> p k t", k=4)),
                 r=(f"ps{pb + 1}",), w=(f"hT{i}b",))

        p1_stageA(0)
        p1_stageA(1)
        for i in range(NTH):
            p1_stageB(i)
            if i + 2 < NTH:
                p1_stageA(i + 2)
        dump("hT", hT[:, :, 1024:1024 + 256], [128, 8, 256], [f"hT{i}{c}" for i in (8, 9) for c in "ab"], BF16)
        R.flush()
    top["B"] = baseB
    if stop_after <= 1:
        return finish()

    with Mark():
        attn_tm = sb("attn_tm", [128, 16, 1024], BF16)
        Wp = [sb(f"Wp{i}", [128, 8, 3, 128], BF16) for i in range(2)]
        QT = [sb(f"QT{i}", [128, 2048], BF16) for i in range(2)]
        KT = [sb(f"KT{i}", [128, 4096], BF16) for i in range(2)]
        Vx = [sb(f"Vx{i}", [128, 32, 2, 65], BF16) for i in range(2)]
        Eb = [sb(f"Eb{i}", [128, 17 * 128], BF16) for i in range(2)]
        exb = [sb(f"exb{i}", [128, 512], BF16) for i in range(4)]
        PT = [sb(f"PT{i}", [128, 512], BF16) for i in range(4)]
        rc = [sb(f"rc{i}", [128, 8]) for i in range(2)]
        for b in range(2):
            for a in range(2):
                R.op("dve", lambda e, b=b, a=a: e.tensor_copy(out=Vx[b][:, :, a, 64], in_=valid[:]), r=("valid",), w=(f"Vx{b}c",))
        nexc = [0]
        for hp in range(8):
            b = hp % 2
            for j in range(3):
                R.dma("pool", lambda e, b=b, j=j, hp=hp: e.dma_start(out=Wp[b][:, :, j, :],
                                                                      in_=w_in[:, :, j * 1024 + hp * 128:j * 1024 + (hp + 1) * 128]),
                      w=(f"Wp{b}",), sem=f"d_Wp{b}")
            for tb in range(4):
                pk = tb % 4

                def mm(e, b=b, tb=tb, pk=pk):
                    ins = None
                    for k in range(8):
                        ins = e.matmul(ps[pk][:, :], lhsT=Wp[b][:, k, 0, :], rhs=hT[:, k, 1024 + tb * 512:1024 + (tb + 1) * 512],
                                       start=(k == 0), stop=(k == 7))
                    return ins
                R.op("pe", mm, r=(f"Wp{b}",), w=(f"ps{pk}",))
                R.op("act", lambda e, b=b, tb=tb, pk=pk: e.activation(out=QT[b][:, tb * 512:(tb + 1) * 512], in_=ps[pk][:, :],
                                                                      func=AF.Copy, scale=0.125), r=(f"ps{pk}",), w=(f"QT{b}",))
            for tb in range(8):
                pk = tb % 4

                def mm(e, b=b, tb=tb, pk=pk):
                    ins = None
                    for k in range(8):
                        ins = e.matmul(ps[pk][:, :], lhsT=Wp[b][:, k, 1, :], rhs=hT[:, k, tb * 512:(tb + 1) * 512],
                                       start=(k == 0), stop=(k == 7))
                    return ins
                R.op("pe", mm, r=(f"Wp{b}",), w=(f"ps{pk}",))
                if tb % 2:
                    R.op("dve", lambda e, b=b, tb=tb, pk=pk: e.tensor_copy(out=KT[b][:, tb * 512:(tb + 1) * 512], in_=ps[pk][:, :]),
                         r=(f"ps{pk}",), w=(f"KT{b}",))
                else:
                    R.op("act", lambda e, b=b, tb=tb, pk=pk: e.activation(out=KT[b][:, tb * 512:(tb + 1) * 512], in_=ps[pk][:, :],
                                                                          func=AF.Copy), r=(f"ps{pk}",), w=(f"KT{b}",))
            for i4 in range(8):
                pk = i4 % 4

                def mm(e, b=b, i4=i4, pk=pk):
                    ins = None
                    for ii in range(4):
                        i = i4 * 4 + ii
                        for k in range(8):
                            ins = e.matmul(ps[pk][:, ii * 128:(ii + 1) * 128], lhsT=hT[:, k, i * 128:(i + 1) * 128],
                                           rhs=Wp[b][:, k, 2, :], start=(k == 0), stop=(k == 7))
                    return ins
                R.op("pe", mm, r=(f"Wp{b}",), w=(f"ps{pk}",))
                for ii in range(4):
                    i = i4 * 4 + ii
                    R.op("dve", lambda e, b=b, i=i, ii=ii, pk=pk: e.tensor_scalar(
                        out=Vx[b][:, i, :, 0:64], in0=ps[pk][:, ii * 128:(ii + 1) * 128].rearrange("p (a e) -> p a e", a=2),
                        scalar1=valid[:, i:i + 1], scalar2=None, op0=ALU.mult), r=(f"ps{pk}", "valid"), w=(f"Vx{b}",))
            if hp == 0:
                dump("QT", QT[0], [128, 2048], ("QT0",), BF16)
                dump("KT", KT[0], [128, 4096], ("KT0",), BF16)
            for a in range(2):
                h = 2 * hp + a
                eb = h % 2
                R.dma("pool", lambda e, h=h, eb=eb: e.dma_start(out=Eb[eb], in_=etab_d[h]), w=(f"Eb{eb}",), sem=f"d_Eb{eb}")
                rows = slice(a * 64, (a + 1) * 64)
                groups = []
                for qi in range(16):
                    for o0 in (0, 4, 8, 12, 16):
                        groups.append((qi, o0, min(4, 17 - o0)))

                def emit_S(gidx, b=b, rows=rows, groups=groups):
                    qi, o0, n = groups[gidx]
                    pk = gidx % 4

                    def mm(e, qi=qi, o0=o0, n=n, pk=pk, b=b, rows=rows):
                        ins = None
                        for j in range(n):
                            ins = e.matmul(ps[pk][:, j * 128:(j + 1) * 128], lhsT=KT[b][rows, (qi + o0 + j) * 128:(qi + o0 + j + 1) * 128],
                                           rhs=QT[b][rows, qi * 128:(qi + 1) * 128], start=True, stop=True)
                        return ins
                    R.op("pe", mm, r=(f"KT{b}", f"QT{b}"), w=(f"ps{pk}",))

                def emit_rest(gidx, b=b, a=a, h=h, eb=eb, groups=groups):
                    qi, o0, n = groups[gidx]
                    pk = gidx % 4
                    x3 = nexc[0] % 4
                    nexc[0] += 1
                    po = 6 + qi % 2
                    R.op("act", lambda e, n=n, pk=pk, x3=x3: e.activation(out=exb[x3][:, 0:n * 128], in_=ps[pk][:, 0:n * 128], func=AF.Exp),
                         r=(f"ps{pk}",), w=(f"exb{x3}",))
                    R.op("dve", lambda e, n=n, o0=o0, x3=x3, eb=eb: e.tensor_tensor(out=PT[x3][:, 0:n * 128], in0=exb[x3][:, 0:n * 128],
                                                                                    in1=Eb[eb][:, o0 * 128:(o0 + n) * 128], op=ALU.mult),
                         r=(f"exb{x3}", f"Eb{eb}"), w=(f"PT{x3}",))

                    def mm(e, qi=qi, o0=o0, n=n, x3=x3, po=po, b=b, a=a):
                        ins = None
                        for j in range(n):
                            o = o0 + j
                            ins = e.matmul(ps[po][:, 0:65], lhsT=PT[x3][:, j * 128:(j + 1) * 128], rhs=Vx[b][:, qi + o, a, :],
                                           start=(o == 0), stop=(o == 16))
                        return ins
                    R.op("pe", mm, r=(f"PT{x3}", f"Vx{b}", f"Vx{b}c"), w=(f"ps{po}",))
                    if o0 == 16:
                        r2 = qi % 2
                        R.op("dve", lambda e, po=po, r2=r2: e.reciprocal(out=rc[r2][:, 0:1], in_=ps[po][:, 64:65]), r=(f"ps{po}",), w=(f"rc{r2}",))
                        R.op("dve", lambda e, po=po, r2=r2, qi=qi, h=h: e.tensor_scalar(
                            out=attn_tm[:, qi, h * 64:(h + 1) * 64], in0=ps[po][:, 0:64], scalar1=rc[r2][:, 0:1], scalar2=None,
                            op0=ALU.mult), r=(f"ps{po}", f"rc{r2}"), w=(f"attn{qi}",))
                LOOK = 3
                for g0 in range(LOOK):
                    emit_S(g0)
                for gidx in range(len(groups)):
                    if gidx + LOOK < len(groups):
                        emit_S(gidx + LOOK)
                    emit_rest(gidx)
        dump("attn0", attn_tm[:, 0, :], [128, 1024], ("attn0",), BF16)
        dump("attn15", attn_tm[:, 15, :], [128, 1024], ("attn15",), BF16)
        R.dma("sp", lambda e: e.dma_start(out=attn_d.rearrange("(i p) f -> p i f", p=128), in_=attn_tm),
              r=tuple(f"attn{q}" for q in range(16)), w=("attn_d",), sem="d_spill")
        R.flush()
    with Mark():
        zs_tm = sb("zs_tm", [128, 16, 1024], BF16)
        Wz = sb("Wz", [128, 8, 1024], BF16)
        R.dma("pool", lambda e: e.dma_start(out=Wz, in_=w_in[:, :, 3072:4096]), w=("Wz",), sem="d_w2")
        for i in range(16):
            for hf in range(2):
                pk = (2 * i + hf) % 4

                def mm(e, i=i, hf=hf, pk=pk):
                    ins = None
                    for k in range(8):
                        ins = e.matmul(ps[pk][:, :], lhsT=hT[:, k, (8 + i) * 128:(9 + i) * 128], rhs=Wz[:, k, hf * 512:(hf + 1) * 512],
                                       start=(k == 0), stop=(k == 7))
                    return ins
                R.op("pe", mm, r=("Wz",), w=(f"ps{pk}",))
                R.op("act", lambda e, i=i, hf=hf, pk=pk: e.activation(out=zs_tm[:, i, hf * 512:(hf + 1) * 512], in_=ps[pk][:, :],
                                                                      func=AF.Silu), r=(f"ps{pk}",), w=(f"zs{i}",))
        R.dma("sp", lambda e: e.dma_start(out=zs_d.rearrange("(i p) f -> p i f", p=128), in_=zs_tm),
              r=tuple(f"zs{q}" for q in range(16)), w=("zs_d",), sem="d_spill")
        R.flush()
    if stop_after <= 2:
        return finish()

    X_tm = sb("X_tm", [128, 16, 1280], BF16)
    BC_fm = sb("BC_fm", [128, 4, 2048], BF16)
    dt_t = sb("dt_t", [128, 16, 32])
    adt = sb("adt", [128, 16, 32])
    eac = sb("eac", [128, 16, 32])
    eEnd = sb("eEnd", [128, 16, 32])
    cdec = sb("cdec", [128, 16, 32])
    dskip = sb("dskip", [128, 16])
    Hloc = [sb(f"Hloc{d}", [128, 1024]) for d in range(2)]
    with Mark():
        Wc = [sb(f"Wc{i}", [128, 8, 128], BF16) for i in range(2)]
        Wdt = sb("Wdt", [128, 8, 32], BF16)
        xsTc = [sb(f"xsTc{i}", [128, 2048], BF16) for i in range(2)]
        craw = sb("craw", [128, 2056])
        cacc = sb("cacc", [128, 2048])
        convw = sb("convw", [128, 12, 5])
        convb = sb("convb", [128, 12])
        dtb = sb("dtb", [128, 32])
        abc = sb("abc", [128, 32])
        dtr = sb("dtr", [128, 16, 32])
        wde = sb("wde", [128, 16, 32])
        xw = [sb(f"xw{i}", [128, 1024], BF16) for i in range(2)]
        bnc_sb = sb("bnc_sb", [128, 32])
        R.dma("pool", lambda e: e.dma_start(out=Wdt, in_=w_in[:, :, 5632:5664]), w=("Wdt",), sem="d_w2")
        R.dma("sp", lambda e: e.dma_start(out=convw, in_=convw_d.rearrange("p (a b) -> p a b", a=12)), w=("convw",), sem="d_c")
        R.dma("sp", lambda e: e.dma_start(out=convb, in_=convb_d), w=("convb",), sem="d_c")
        R.dma("sp", lambda e: e.dma_start(out=dtb, in_=dtb_row.partition_broadcast(128)), w=("dtb",), sem="d_c")
        R.dma("sp", lambda e: e.dma_start(out=abc, in_=alog_row.partition_broadcast(128)), w=("abc0",), sem="d_c")
        R.dma("sp", lambda e: e.dma_start(out=dskip, in_=dskip_row.partition_broadcast(128)), w=("dskip",), sem="d_c")
        R.op("act", lambda e: e.activation(out=abc, in_=abc, func=AF.Exp), r=("abc0",), w=("abc1",))
        R.op("dve", lambda e: e.tensor_scalar(out=abc, in0=abc, scalar1=-1.0, scalar2=None, op0=ALU.mult), r=("abc1",), w=("abc",))
        pb5 = ps[5][:, :].bitcast(BF16)
        pb6 = ps[6][:, :].bitcast(BF16)
        for c in range(12):
            wb = c % 2
            R.dma("pool", lambda e, c=c, wb=wb: e.dma_start(out=Wc[wb], in_=w_in[:, :, 4096 + c * 128:4096 + (c + 1) * 128]),
                  w=(f"Wc{wb}",), sem=f"d_Wc{wb}")
            for blk in range(4):
                def mm(e, wb=wb, blk=blk):
                    ins = None
                    for k in range(8):
                        ins = e.matmul(ps[blk][:, :], lhsT=Wc[wb][:, k, :], rhs=hT[:, k, 1024 + blk * 512:1024 + (blk + 1) * 512],
                                       start=(k == 0), stop=(k == 7))
                    return ins
                R.op("pe", mm, r=(f"Wc{wb}",), w=(f"ps{blk}",))

            def mmh(e, wb=wb, c=c):
                ins = None
                for side, t0 in ((0, 1022), (1, 3072)):
                    for k in range(8):
                        ins = e.matmul(ps[4][:, c * 4 + side * 2:c * 4 + side * 2 + 2], lhsT=Wc[wb][:, k, :], rhs=hT[:, k, t0:t0 + 2],
                                       start=(k == 0), stop=(k == 7))
                return ins
            R.op("pe", mmh, r=(f"Wc{wb}",), w=("ps4",))
            for blk in range(4):
                dst = craw[:, 2 + blk * 512:2 + (blk + 1) * 512]
                if blk < 2:
                    R.op("act", lambda e, blk=blk, dst=dst: e.activation(out=dst, in_=ps[blk][:, :], func=AF.Copy),
                         r=(f"ps{blk}",), w=(f"craw{blk}",))
                else:
                    R.op("dve", lambda e, blk=blk, dst=dst: e.tensor_copy(out=dst, in_=ps[blk][:, :]), r=(f"ps{blk}",), w=(f"craw{blk}",))
            R.op("dve", lambda e, c=c: e.tensor_scalar(out=craw[:, 0:2], in0=ps[4][:, c * 4:c * 4 + 2], scalar1=edge[:, 0:1], scalar2=None,
                                                       op0=ALU.mult), r=("ps4", "edge"), w=("crawh0",))
            R.op("dve", lambda e, c=c: e.tensor_scalar(out=craw[:, 2050:2052], in0=ps[4][:, c * 4 + 2:c * 4 + 4], scalar1=edge[:, 1:2],
                                                       scalar2=None, op0=ALU.mult), r=("ps4", "edge"), w=("crawh1",))
            ck = [f"craw{j}" for j in range(4)] + ["crawh0", "crawh1"]
            R.op("dve", lambda e, c=c: e.tensor_scalar(out=cacc, in0=craw[:, 0:2048], scalar1=convw[:, c, 0:1], scalar2=None, op0=ALU.mult),
                 r=ck + ["convw"], w=("cacc",))
            for i in range(1, 5):
                R.op("dve", lambda e, c=c, i=i: e.scalar_tensor_tensor(out=cacc, in0=craw[:, i:i + 2048], scalar=convw[:, c, i:i + 1], in1=cacc,
                                                                       op0=ALU.mult, op1=ALU.add), r=ck + ["cacc"], w=("cacc",))
            if c < 8:
                dst, dkey = xsTc[c % 2], f"xsTc{c % 2}"
            else:
                dst, dkey = BC_fm[:, c - 8, :], f"BCfm{c - 8}"
            R.op("act", lambda e, c=c, dst=dst: e.activation(out=dst, in_=cacc, func=AF.Silu, bias=convb[:, c:c + 1]),
                 r=("cacc", "convb"), w=(dkey,))
            if c == 0:
                dump("xbcT0", xsTc[0], [128, 2048], ("xsTc0",), BF16)
            if c == 9:
                dump("xbcT9", BC_fm[:, 1, :], [128, 2048], ("BCfm1",), BF16)
            if c < 10:
                def tr(e, dst=dst):
                    ins = None
                    for i in range(16):
                        pbv = pb5 if i < 8 else pb6
                        ins = e.transpose(out=pbv[:, (i % 8) * 128:(i % 8 + 1) * 128], in_=dst[:, i * 128:(i + 1) * 128], identity=identb[:])
                    return ins
                R.op("pe", tr, r=(dkey, "identb"), w=("ps5", "ps6"))
                R.op("act", lambda e, c=c: e.activation(out=X_tm[:, 0:8, c * 128:(c + 1) * 128],
                                                        in_=pb5[:, 0:1024].rearrange("p (i f) -> p i f", i=8), func=AF.Copy),
                     r=("ps5",), w=(f"Xtm_{c}a",))
                R.op("dve", lambda e, c=c: e.tensor_copy(out=X_tm[:, 8:16, c * 128:(c + 1) * 128],
                                                         in_=pb6[:, 0:1024].rearrange("p (i f) -> p i f", i=8)),
                     r=("ps6",), w=(f"Xtm_{c}b",))
        xkeys = [f"Xtm_{c}{s}" for c in range(10) for s in "ab"]
        dump("Xtm3", X_tm[:, 3, :], [128, 1280], xkeys, BF16)

        def mmdt(e):
            ins = None
            for i in range(16):
                for k in range(8):
                    ins = e.matmul(ps[7][:, i * 32:(i + 1) * 32], lhsT=hT[:, k, (8 + i) * 128:(9 + i) * 128], rhs=Wdt[:, k, :],
                                   start=(k == 0), stop=(k == 7))
            return ins
        R.op("pe", mmdt, r=("Wdt",), w=("ps7",))
        R.op("dve", lambda e: e.tensor_tensor(out=dtr, in0=ps[7][:, :].rearrange("p (c h) -> p c h", c=16),
                                              in1=dtb.unsqueeze(1).to_broadcast([128, 16, 32]), op=ALU.add), r=("ps7", "dtb"), w=("dtr",))
        R.op("act", lambda e: e.activation(out=dtr, in_=dtr, func=AF.Exp), r=("dtr",), w=("dtr",))
        R.op("act", lambda e: e.activation(out=dt_t, in_=dtr, func=AF.Ln, bias=1.0), r=("dtr",), w=("dt_t",))
        R.op("dve", lambda e: e.tensor_tensor(out=adt, in0=dt_t, in1=abc.unsqueeze(1).to_broadcast([128, 16, 32]), op=ALU.mult),
             r=("dt_t", "abc"), w=("adt",))
        dump("dt", dt_t, [128, 16, 32], ("dt_t",))
        adt2 = adt.rearrange("p c h -> p (c h)")
        for j, m in enumerate((triLE, triGE, mSU, mSL, ones)):
            R.op("pe", lambda e, j=j, m=m: e.matmul(ps[j][:, :], lhsT=m, rhs=adt2, start=True, stop=True), r=("adt", "masks"), w=(f"ps{j}",))

        def pv3(j):
            return ps[j][:, :].rearrange("p (c h) -> p c h", c=16)
        for d in range(2):
            hs = slice(d * 16, (d + 1) * 16)
            R.op("act", lambda e, d=d, hs=hs: e.activation(out=eac[:, :, hs], in_=pv3(d)[:, :, hs], func=AF.Exp), r=(f"ps{d}",), w=(f"eac{d}",))
            R.op("act", lambda e, d=d, hs=hs: e.activation(out=eEnd[:, :, hs], in_=pv3(2 + d)[:, :, hs], func=AF.Exp), r=(f"ps{2 + d}",),
                 w=(f"eEnd{d}",))
        R.op("act", lambda e: e.activation(out=cdec, in_=pv3(4), func=AF.Exp), r=("ps4",), w=("cdec",))
        R.op("dve", lambda e: e.tensor_copy(out=bnc_sb, in_=cdec[:, 0, :]), r=("cdec",), w=("bnc_sb",))
        for c in range(1, 16):
            R.op("dve", lambda e, c=c: e.tensor_tensor(out=bnc_sb, in0=bnc_sb, in1=cdec[:, c, :], op=ALU.mult), r=("bnc_sb", "cdec"), w=("bnc_sb",))

        R.op("dve", lambda e: e.tensor_tensor(out=wde, in0=dt_t, in1=eEnd, op=ALU.mult), r=("dt_t", "eEnd0", "eEnd1"), w=("wde",))
        for d in range(2):
            R.op("pool", lambda e, d=d: e.memset(Hloc[d], 0.0), w=(f"H{d}",))
            order = range(16) if d == 0 else range(15, -1, -1)
            for n_i, c in enumerate(order):
                b = n_i % 2
                R.op("dve", lambda e, c=c, d=d, b=b: e.tensor_tensor(
                    out=v3(xw[b]), in0=v3(X_tm[:, c, 0:1024]),
                    in1=wde[:, c, d * 16:(d + 1) * 16].unsqueeze(2).to_broadcast([128, 16, 64]), op=ALU.mult),
                    r=xkeys + ["wde"], w=(f"xw{b}",))
                pp = 2 * b

                def mms(e, c=c, b=b, pp=pp):
                    ins = None
                    for g in range(2):
                        ins = e.matmul(ps[pp + g][:, :], lhsT=X_tm[:, c, 1024 + g * 128:1024 + (g + 1) * 128],
                                       rhs=xw[b][:, g * 512:(g + 1) * 512], start=True, stop=True)
                    return ins
                R.op("pe", mms, r=[f"xw{b}"] + xkeys, w=(f"ps{pp}", f"ps{pp + 1}"))
                R.op("dve", lambda e, c=c, d=d: e.tensor_tensor(
                    out=v3(Hloc[d]), in0=v3(Hloc[d]),
                    in1=cdec[:, c, d * 16:(d + 1) * 16].unsqueeze(2).to_broadcast([128, 16, 64]), op=ALU.mult),
                    r=(f"H{d}", "cdec"), w=(f"H{d}",))
                for g in range(2):
                    R.op("dve", lambda e, d=d, g=g, pp=pp: e.tensor_tensor(
                        out=Hloc[d][:, g * 512:(g + 1) * 512], in0=Hloc[d][:, g * 512:(g + 1) * 512], in1=ps[pp + g][:, :],
                        op=ALU.add), r=(f"H{d}", f"ps{pp + g}"), w=(f"H{d}",))
        dump("Hloc0", Hloc[0], [128, 1024], ("H0",))
        dump("Hloc1", Hloc[1], [128, 1024], ("H1",))
        for d in range(2):
            R.dma("sp", lambda e, d=d: e.dma_start(out=bnc_in[:, d * 1024:(d + 1) * 1024], in_=Hloc[d]), r=(f"H{d}",),
                  w=("bnc_in",), sem="d_bnc")
        R.dma("sp", lambda e: e.dma_start(out=bnc2_in[:, 0:32], in_=bnc_sb), r=("bnc_sb",), w=("bnc2_in",), sem="d_bnc")
        R.dma("sp", lambda e: e.dma_start(out=bnc2_in[:, 32:64], in_=bnc_sb), r=("bnc_sb",), w=("bnc2_in",), sem="d_bnc")
        R.op("pool", lambda e: e.collective_compute("AllGather", ALU.bypass, replica_groups=[[0, 1, 2, 3], [4, 5, 6, 7]],
                                                   ins=[bnc_in], outs=[bnc_out]),
             r=("bnc_in",), w=("bnc_out",), semkey="cc", inc=None)
        R.op("pool", lambda e: e.collective_compute("AllGather", ALU.bypass, replica_groups=[[0, 1, 2, 3], [4, 5, 6, 7]],
                                                   ins=[bnc2_in], outs=[bnc2_out]),
             r=("bnc2_in",), w=("bnc2_out",), semkey="cc", inc=None)
        R.op("pool", lambda e: e.memset(bnc_sb[:, 0:1], 0.0), r=("bnc_out", "bnc2_out", "bnc2_in"), w=("ccdone",))
        R.flush()
    if stop_after <= 3:
        return finish()

    top["A"] = 0
    yacc = sb("yacc", [128, 16, 1024], BF16, ar="A")
    Hin = [sb(f"Hin{d}", [128, 1024], ar="A") for d in range(2)]
    with Mark():
        gath = sb("gath", [128, 4, 2048])
        gath2 = sb("gath2", [128, 4, 64])
        Pc = sb("Pc", [128, 1024])
        R.dma("sp", lambda e: e.dma_start(out=gath, in_=bnc_out.rearrange("(m p) n -> p m n", p=128)), r=("bnc_out",), w=("gath",), sem="d_g")
        R.dma("sp", lambda e: e.dma_start(out=gath2, in_=bnc2_out.rearrange("(m p) n -> p m n", p=128)), r=("bnc2_out",), w=("gath",), sem="d_g")
        for d in range(2):
            R.op("pool", lambda e, d=d: e.memset(Hin[d], 0.0), w=(f"Hin{d}",))
            R.op("pool", lambda e: e.memset(Pc, 0.0), w=("Pc",))
            seq = [0, 1, 2] if d == 0 else [3, 2, 1]
            for m in seq:
                tgt = m + 1 if d == 0 else m - 1
                R.op("dve", lambda e, m=m, d=d: e.tensor_tensor(
                    out=v3(Pc), in0=v3(Pc), in1=gath2[:, m, d * 16:(d + 1) * 16].unsqueeze(2).to_broadcast([128, 16, 64]),
                    op=ALU.mult), r=("Pc", "gath"), w=("Pc",))
                R.op("dve", lambda e, m=m, d=d: e.tensor_tensor(out=Pc, in0=Pc, in1=gath[:, m, d * 1024:(d + 1) * 1024], op=ALU.add),
                     r=("Pc", "gath"), w=("Pc",))
                R.op("dve", lambda e, d=d, tgt=tgt: e.scalar_tensor_tensor(out=Hin[d], in0=Pc, scalar=sel[:, tgt:tgt + 1], in1=Hin[d],
                                                                          op0=ALU.mult, op1=ALU.add), r=("Pc", "sel", f"Hin{d}"), w=(f"Hin{d}",))
        dump("Hin0", Hin[0], [128, 1024], ("Hin0",))
        dump("Hin1", Hin[1], [128, 1024], ("Hin1",))
        R.flush()
    with Mark(), Mark("A"):
        ytmp = [sb(f"ytmp{i}", [128, 1024], ar="A") for i in range(2)]
        ytot = [sb(f"ytot{i}", [128, 1024], ar="A") for i in range(2)]
        exs = [sb(f"exs{i}", [128, 16, 128], BF16, ar="A") for i in range(2)]
        Rm = [sb(f"Rm{i}", [128, 16, 128]) for i in range(2)]
        MT = [sb(f"MT{i}", [128, 16, 128], BF16) for i in range(2)]
        CBm = [sb(f"CBm{i}", [128, 2, 128], BF16) for i in range(2)]
        xd = [sb(f"xd{i}", [128, 1024], BF16) for i in range(2)]
        xw2 = [sb(f"xw2{i}", [128, 1024], BF16) for i in range(2)]
        Hb = sb("Hb", [128, 1024], BF16)
        gsq = sb("gsq", [128, 1024], BF16)
        wde2 = sb("wde2", [128, 16, 32])
        ssg = sb("ssg", [128, 16])
        tg = sb("tg", [128, 16])
        sg_ = sb("sg_", [128, 16])
        rg = sb("rg", [128, 16])
        zsb = [sb(f"zsb{i}", [128, 1024], BF16) for i in range(2)]
        R.op("dve", lambda e: e.tensor_tensor(out=wde2, in0=dt_t, in1=eEnd, op=ALU.mult), w=("wde2",))
        for d in range(2):
            hs = slice(d * 16, (d + 1) * 16)
            tri = triLE if d == 0 else triGE
            sm = mSU if d == 0 else mSL
            H = Hin[d]
            R.op("act", lambda e, H=H: e.activation(out=Hb, in_=H, func=AF.Copy), r=(f"Hin{d}",), w=("Hb",))
            order = list(range(16)) if d == 0 else list(range(15, -1, -1))

            def front(n_i, d=d, hs=hs, tri=tri, sm=sm, order=order):
                c = order[n_i]
                p = n_i % 2
                cs_ = slice(c * 128, (c + 1) * 128)
                if d == 1:
                    R.dma("sp", lambda e: e.dma_start(out=zsb[p], in_=zs_d[c * 128:(c + 1) * 128, :]), r=("zs_d",),
                          w=(f"zsb{p}",), sem=f"d_zs{p}")

                def mmcb(e):
                    ins = None
                    for g in range(2):
                        ins = e.matmul(ps[0][:, g * 128:(g + 1) * 128], lhsT=BC_fm[:, g, cs_], rhs=BC_fm[:, 2 + g, cs_], start=True, stop=True)
                    return ins
                R.op("pe", mmcb, w=("ps0",))
                R.op("dve", lambda e: e.tensor_tensor(out=CBm[p], in0=ps[0][:, 0:256].rearrange("p (g l) -> p g l", g=2),
                                                      in1=tri.unsqueeze(1).to_broadcast([128, 2, 128]), op=ALU.mult),
                     r=("ps0",), w=(f"CBm{p}",))
                R.op("pool", lambda e: e.tensor_tensor(
                    out=Rm[p], in0=adt[:, c, hs].unsqueeze(2).to_broadcast([128, 16, 128]),
                    in1=tri.unsqueeze(1).to_broadcast([128, 16, 128]), op=ALU.mult), w=(f"Rm{p}",))
                Rm2 = Rm[p].rearrange("p h l -> p (h l)")

                def mmseg(e):
                    ins = None
                    for j in range(4):
                        ins = e.matmul(ps[1 + j][:, :], lhsT=sm, rhs=Rm2[:, j * 512:(j + 1) * 512], start=True, stop=True)
                    return ins
                R.op("pe", mmseg, r=(f"Rm{p}",), w=("ps1", "ps2", "ps3", "ps4"))
                for j in range(4):
                    R.op("act", lambda e, j=j: e.activation(out=exs[p][:, j * 4:(j + 1) * 4, :],
                                                            in_=ps[1 + j][:, :].rearrange("p (h l) -> p h l", h=4), func=AF.Exp),
                         r=(f"ps{1 + j}",), w=(f"exs{p}_{j}",))
                R.op("dve", lambda e: e.tensor_tensor(out=v3(xd[p]), in0=v3(X_tm[:, c, 0:1024]),
                                                      in1=dt_t[:, c, hs].unsqueeze(2).to_broadcast([128, 16, 64]), op=ALU.mult),
                     w=(f"xd{p}",))
                R.op("dve", lambda e: e.tensor_tensor(out=v3(xw2[p]), in0=v3(X_tm[:, c, 0:1024]),
                                                      in1=wde2[:, c, hs].unsqueeze(2).to_broadcast([128, 16, 64]), op=ALU.mult),
                     r=("wde2",), w=(f"xw2{p}",))
                for g in range(2):
                    R.op("dve", lambda e, g=g: e.tensor_tensor(out=MT[p][:, g * 8:(g + 1) * 8, :], in0=exs[p][:, g * 8:(g + 1) * 8, :],
                                                              in1=CBm[p][:, g:g + 1, :].to_broadcast([128, 8, 128]), op=ALU.mult),
                         r=(f"exs{p}_{2 * g}", f"exs{p}_{2 * g + 1}", f"CBm{p}"), w=(f"MT{p}_{g}",))

            def tail(n_i, d=d, hs=hs, H=H, order=order):
                c = order[n_i]
                p = n_i % 2
                cs_ = slice(c * 128, (c + 1) * 128)

                def mmy(e):
                    ins = None
                    for h in range(16):
                        ins = e.matmul(ps[5 + h // 8][:, (h % 8) * 64:(h % 8 + 1) * 64], lhsT=MT[p][:, h, :], rhs=xd[p][:, h * 64:(h + 1) * 64],
                                       start=True, stop=True)
                    return ins
                R.op("pe", mmy, r=(f"MT{p}_0", f"MT{p}_1", f"xd{p}"), w=("ps5", "ps6"))
                for g in range(2):
                    gs = slice(g * 512, (g + 1) * 512)
                    R.op("pe", lambda e, g=g: e.matmul(ps[7][:, :], lhsT=BC_fm[:, 2 + g, cs_], rhs=Hb[:, g * 512:(g + 1) * 512], start=True, stop=True),
                         r=("Hb",), w=("ps7",))
                    R.op("dve", lambda e, g=g, gs=gs: e.tensor_tensor(
                        out=v3(ytmp[p][:, gs]), in0=v3(ps[7][:, :]),
                        in1=eac[:, c, d * 16 + g * 8:d * 16 + (g + 1) * 8].unsqueeze(2).to_broadcast([128, 8, 64]), op=ALU.mult),
                        r=("ps7",), w=(f"ytmp{p}_{g}",))
                R.op("dve", lambda e: e.tensor_tensor(out=v3(H), in0=v3(H), in1=cdec[:, c, d * 16:(d + 1) * 16].unsqueeze(2).to_broadcast([128, 16, 64]),
                                                      op=ALU.mult), r=(f"Hin{d}", "Hb"), w=(f"Hin{d}",))
                for g in range(2):
                    R.op("pe", lambda e, g=g: e.matmul(ps[7][:, :], lhsT=X_tm[:, c, 1024 + g * 128:1024 + (g + 1) * 128],
                                                       rhs=xw2[p][:, g * 512:(g + 1) * 512], start=True, stop=True),
                         r=(f"xw2{p}",), w=("ps7",))
                    R.op("dve", lambda e, g=g: e.tensor_tensor(out=H[:, g * 512:(g + 1) * 512], in0=H[:, g * 512:(g + 1) * 512],
                                                              in1=ps[7][:, :], op=ALU.add), r=(f"Hin{d}", "ps7"), w=(f"Hin{d}",))
                R.op("act", lambda e: e.activation(out=Hb, in_=H, func=AF.Copy), r=(f"Hin{d}",), w=("Hb",))
                for g in range(2):
                    gs = slice(g * 512, (g + 1) * 512)
                    if d == 0:
                        R.op("dve", lambda e, g=g, gs=gs: e.tensor_tensor(out=yacc[:, c, gs], in0=ps[5 + g][:, :], in1=ytmp[p][:, gs], op=ALU.add),
                             r=(f"ps{5 + g}", f"ytmp{p}_{g}"), w=(f"yacc{c}_{g}",))
                    else:
                        R.op("dve", lambda e, g=g, gs=gs: e.tensor_tensor(out=ytot[p][:, gs], in0=ps[5 + g][:, :], in1=ytmp[p][:, gs], op=ALU.add),
                             r=(f"ps{5 + g}", f"ytmp{p}_{g}"), w=(f"ytot{p}_{g}",))
                        R.op("dve", lambda e, gs=gs: e.tensor_tensor(out=ytot[p][:, gs], in0=ytot[p][:, gs], in1=yacc[:, c, gs], op=ALU.add),
                             r=(f"ytot{p}_{g}", f"yacc{c}_{g}"), w=(f"ytot{p}_{g}",))
                        R.op("dve", lambda e, g=g, gs=gs: e.tensor_tensor(
                            out=v3(ytmp[p][:, gs]), in0=v3(X_tm[:, c, gs]),
                            in1=dskip[:, g * 8:(g + 1) * 8].unsqueeze(2).to_broadcast([128, 8, 64]), op=ALU.mult),
                            r=(f"ytot{p}_{g}",), w=(f"ytmp{p}_{g}",))
                        R.op("dve", lambda e, gs=gs: e.tensor_tensor(out=ytot[p][:, gs], in0=ytot[p][:, gs], in1=ytmp[p][:, gs], op=ALU.add),
                             r=(f"ytmp{p}_{g}", f"ytot{p}_{g}"), w=(f"ytot{p}_{g}",))
                        R.op("dve", lambda e, gs=gs: e.tensor_tensor(out=ytot[p][:, gs], in0=ytot[p][:, gs], in1=zsb[p][:, gs], op=ALU.mult),
                             r=(f"ytot{p}_{g}", f"zsb{p}"), w=(f"ytot{p}_{g}",))
                if d == 1:
                    yk = (f"ytot{p}_0", f"ytot{p}_1")
                    R.op("act", lambda e: e.activation(out=gsq, in_=ytot[p], func=AF.Square, accum_out=ssg[:, c:c + 1]),
                         r=yk, w=("gsq", f"ssg{c}"))
                    R.op("dve", lambda e: e.tensor_scalar(out=tg[:, c:c + 1], in0=ssg[:, c:c + 1], scalar1=1.0 / 1024, scalar2=EPS,
                                                          op0=ALU.mult, op1=ALU.add), r=(f"ssg{c}",), w=(f"tg{c}",))
                    R.op("act", lambda e: e.activation(out=sg_[:, c:c + 1], in_=tg[:, c:c + 1], func=AF.Sqrt), r=(f"tg{c}",), w=(f"sg{c}",))
                    R.op("dve", lambda e: e.reciprocal(out=rg[:, c:c + 1], in_=sg_[:, c:c + 1]), r=(f"sg{c}",), w=(f"rg{c}",))
                    R.op("dve", lambda e: e.scalar_tensor_tensor(out=yacc[:, c, :], in0=ytot[p], scalar=rg[:, c:c + 1], in1=gssd,
                                                                 op0=ALU.mult, op1=ALU.mult),
                         r=yk + (f"rg{c}", "gssd", f"yacc{c}_0", f"yacc{c}_1"), w=(f"yn{c}",))

            front(0)
            for n_i in range(16):
                if n_i + 1 < 16:
                    front(n_i + 1)
                tail(n_i)
            if d == 0:
                dump("yF0", yacc[:, 0, :], [128, 1024], ("yacc0_0", "yacc0_1"), BF16)
                dump("yF5", yacc[:, 5, :], [128, 1024], ("yacc5_0", "yacc5_1"), BF16)
        dump("yn3", yacc[:, 3, :], [128, 1024], ("yn3",), BF16)
        dump("yn12", yacc[:, 12, :], [128, 1024], ("yn12",), BF16)
        R.flush()
    top["B"] = baseB
    if stop_after <= 4:
        return finish()

    h2T = sb("h2T", [128, 8, 2048], BF16)
    combT = sb("combT", [32, 2048])
    top["A"] = 8192
    with Mark(), Mark("A"):
        Wout = sb("Wout", [128, 16, 1024], BF16, ar="A")
        Wr = sb("Wr", [128, 8, 32])
        brt = sb("brt", [128, 32])
        catT = [sb(f"catT{i}", [128, 16, 128], BF16) for i in range(2)]
        xt = [sb(f"xt5_{i}", [128, 1024]) for i in range(2)]
        at = [sb(f"at5_{i}", [128, 1024], BF16) for i in range(2)]
        r1 = [sb(f"r1_{i}", [128, 1024]) for i in range(2)]
        hm2 = [sb(f"hm2_{i}", [128, 1024]) for i in range(2)]
        h32 = [sb(f"h32_{i}", [128, 8, 128]) for i in range(2)]
        junk = sb("junk5", [128, 1024], BF16)
        sv = sb("sv5", [128, 16, 8])
        lg = [sb(f"lg{i}", [128, 32]) for i in range(2)]
        mx8 = [sb(f"mx8_{i}", [128, 8]) for i in range(2)]
        msk = [sb(f"msk{i}", [128, 32]) for i in range(2)]
        exr = [sb(f"exr{i}", [128, 32]) for i in range(2)]
        cmb = [sb(f"cmb{i}", [128, 32]) for i in range(2)]
        R.dma("pool", lambda e: e.dma_start(out=Wout, in_=w_out), w=("Wout",), sem="d_w2")
        R.dma("sp", lambda e: e.dma_start(out=Wr, in_=w_router), w=("Wr",), sem="d_c")
        R.dma("sp", lambda e: e.dma_start(out=brt, in_=b_router_row.partition_broadcast(128)), w=("brt",), sem="d_c")
        pb0 = ps[0][:, :].bitcast(BF16)
        pb1 = ps[1][:, :].bitcast(BF16)
        def p5_stageA(i):
            b = i % 2
            R.dma("sp", lambda e, i=i, b=b: e.dma_start(out=xt[b], in_=xh[(8 + i) * 128:(9 + i) * 128, :]), w=(f"xt{b}",), sem=f"d_xt{b}")
            R.dma("sp", lambda e, i=i, b=b: e.dma_start(out=at[b], in_=attn_d[i * 128:(i + 1) * 128, :]), r=("attn_d",), w=(f"at{b}",), sem=f"d_at{b}")

            def tra(e, b=b):
                ins = None
                for f in range(8):
                    ins = e.transpose(out=pb0[:, f * 128:(f + 1) * 128], in_=at[b][:, f * 128:(f + 1) * 128], identity=identb[:])
                return ins
            R.op("pe", tra, r=(f"at{b}", "identb"), w=("ps0",))
            R.op("act", lambda e, b=b: e.activation(out=catT[b][:, 0:8, :].rearrange("p f t -> p (f t)"), in_=pb0[:, 0:1024], func=AF.Copy),
                 r=("ps0",), w=(f"catT{b}a",))

            def try_(e, i=i):
                ins = None
                for f in range(8):
                    ins = e.transpose(out=pb1[:, f * 128:(f + 1) * 128], in_=yacc[:, i, f * 128:(f + 1) * 128], identity=identb[:])
                return ins
            R.op("pe", try_, w=("ps1",))
            R.op("dve", lambda e, b=b: e.tensor_copy(out=catT[b][:, 8:16, :].rearrange("p f t -> p (f t)"), in_=pb1[:, 0:1024]),
                 r=("ps1",), w=(f"catT{b}b",))

            def mmo(e, b=b):
                ins = None
                for hf in range(2):
                    for k in range(16):
                        ins = e.matmul(ps[2 + hf][:, :], lhsT=catT[b][:, k, :], rhs=Wout[:, k, hf * 512:(hf + 1) * 512],
                                       start=(k == 0), stop=(k == 15))
                return ins
            R.op("pe", mmo, r=(f"catT{b}a", f"catT{b}b", "Wout"), w=("ps2", "ps3"))
            for hf in range(2):
                R.op("act", lambda e, i=i, hf=hf: e.activation(out=junk[:, hf * 512:(hf + 1) * 512], in_=ps[2 + hf][:, :], func=AF.Square,
                                                               accum_out=sv[:, i, hf:hf + 1]), r=(f"ps{2 + hf}",), w=(f"junk{hf}", f"sv{i}_{hf}"))
            R.op("dve", lambda e, i=i: e.tensor_tensor(out=sv[:, i, 2:3], in0=sv[:, i, 0:1], in1=sv[:, i, 1:2], op=ALU.add),
                 r=(f"sv{i}_0", f"sv{i}_1"), w=(f"sv{i}_2",))
            R.op("dve", lambda e, i=i: e.tensor_scalar(out=sv[:, i, 3:4], in0=sv[:, i, 2:3], scalar1=1.0 / 1024, scalar2=EPS, op0=ALU.mult,
                                                       op1=ALU.add), r=(f"sv{i}_2",), w=(f"sv{i}_3",))
            R.op("act", lambda e, i=i: e.activation(out=sv[:, i, 4:5], in_=sv[:, i, 3:4], func=AF.Sqrt), r=(f"sv{i}_3",), w=(f"sv{i}_4",))
            R.op("dve", lambda e, i=i: e.reciprocal(out=sv[:, i, 5:6], in_=sv[:, i, 4:5]), r=(f"sv{i}_4",), w=(f"sv{i}_5",))
            for hf in range(2):
                hsl = slice(hf * 512, (hf + 1) * 512)
                R.op("dve", lambda e, i=i, b=b, hf=hf, hsl=hsl: e.scalar_tensor_tensor(out=r1[b][:, hsl], in0=ps[2 + hf][:, :], scalar=sv[:, i, 5:6],
                                                                                      in1=Gm[:, hsl], op0=ALU.mult, op1=ALU.mult),
                     r=(f"ps{2 + hf}", f"sv{i}_5", f"junk{hf}"), w=(f"r1_{b}_{hf}",))
                R.op("dve", lambda e, b=b, hsl=hsl: e.tensor_tensor(out=r1[b][:, hsl], in0=r1[b][:, hsl], in1=xt[b][:, hsl], op=ALU.add),
                     r=(f"r1_{b}_{hf}", f"xt{b}"), w=(f"r1_{b}_{hf}",))
            rk = (f"r1_{b}_0", f"r1_{b}_1")
            R.dma("sp", lambda e, i=i, b=b: e.dma_start(out=res1_d[i * 128:(i + 1) * 128, :], in_=r1[b]), r=rk, w=(f"res1d{i}",), sem=f"d_r1{b}")
            if i == 2:
                dump("res1_2", r1[b], [128, 1024], rk)
            R.op("act", lambda e, i=i, b=b: e.activation(out=junk, in_=r1[b], func=AF.Square, accum_out=sv[:, i, 6:7]),
                 r=rk, w=("junk0", "junk1", f"sv{i}_6"))
            R.op("dve", lambda e, i=i: e.tensor_scalar(out=sv[:, i, 7:8], in0=sv[:, i, 6:7], scalar1=1.0 / 1024, scalar2=EPS, op0=ALU.mult,
                                                       op1=ALU.add), r=(f"sv{i}_6",), w=(f"sv{i}_7",))
            R.op("act", lambda e, i=i: e.activation(out=sv[:, i, 6:7], in_=sv[:, i, 7:8], func=AF.Sqrt), r=(f"sv{i}_7",), w=(f"sv{i}_6",))
            R.op("dve", lambda e, i=i: e.reciprocal(out=sv[:, i, 7:8], in_=sv[:, i, 6:7]), r=(f"sv{i}_6",), w=(f"sv{i}_7",))
            R.op("dve", lambda e, i=i, b=b: e.scalar_tensor_tensor(out=hm2[b], in0=r1[b], scalar=sv[:, i, 7:8], in1=Arow_f, op0=ALU.mult, op1=ALU.mult),
                 r=rk + (f"sv{i}_7",), w=(f"hm2{b}",))
            R.op("dve", lambda e, b=b: e.tensor_tensor(out=hm2[b], in0=hm2[b], in1=Brow_f, op=ALU.add), r=(f"hm2{b}",), w=(f"hm2{b}",))

            def mmt(e, b=b):
                ins = None
                for k in range(8):
                    ins = e.matmul(ps[4 + k // 4][:, (k % 4) * 128:(k % 4 + 1) * 128], lhsT=hm2[b][:, k * 128:(k + 1) * 128], rhs=ident[:],
                                   start=True, stop=True)
                return ins
            R.op("pe", mmt, r=(f"hm2{b}",), w=("ps4", "ps5"))
            p4v = ps[4][:, :].rearrange("p (k t) -> p k t", k=4)
            p5v = ps[5][:, :].rearrange("p (k t) -> p k t", k=4)
            R.op("act", lambda e, i=i, p4v=p4v: e.activation(out=h2T[:, 0:4, i * 128:(i + 1) * 128], in_=p4v, func=AF.Copy), r=("ps4",), w=(f"h2T{i}a",))
            R.op("act", lambda e, b=b, p4v=p4v: e.activation(out=h32[b][:, 0:4, :], in_=p4v, func=AF.Copy), r=("ps4",), w=(f"h32_{b}a",))
            R.op("dve", lambda e, i=i, p5v=p5v: e.tensor_copy(out=h2T[:, 4:8, i * 128:(i + 1) * 128], in_=p5v), r=("ps5",), w=(f"h2T{i}b",))
            R.op("dve", lambda e, b=b, p5v=p5v: e.tensor_copy(out=h32[b][:, 4:8, :], in_=p5v), r=("ps5",), w=(f"h32_{b}b",))

        def p5_stageB(i):
            b = i % 2
            def mmr(e, b=b):
                ins = None
                for k in range(8):
                    ins = e.matmul(ps[6][:, 0:32], lhsT=h32[b][:, k, :], rhs=Wr[:, k, :], start=(k == 0), stop=(k == 7))
                return ins
            R.op("pe", mmr, r=(f"h32_{b}a", f"h32_{b}b", "Wr"), w=("ps6",))
            R.op("dve", lambda e, b=b: e.tensor_tensor(out=lg[b], in0=ps[6][:, 0:32], in1=brt, op=ALU.add), r=("ps6", "brt"), w=(f"lg{b}",))
            R.op("dve", lambda e, b=b: e.max(out=mx8[b], in_=lg[b]), r=(f"lg{b}",), w=(f"mx8{b}",))
            R.op("dve", lambda e, b=b: e.tensor_scalar(out=msk[b], in0=lg[b], scalar1=mx8[b][:, 3:4], scalar2=None, op0=ALU.is_ge),
                 r=(f"lg{b}", f"mx8{b}"), w=(f"msk{b}",))
            R.op("dve", lambda e, b=b: e.tensor_scalar(out=exr[b], in0=lg[b], scalar1=mx8[b][:, 0:1], scalar2=None, op0=ALU.subtract),
                 r=(f"lg{b}", f"mx8{b}"), w=(f"exr{b}",))
            R.op("act", lambda e, b=b: e.activation(out=exr[b], in_=exr[b], func=AF.Exp), r=(f"exr{b}",), w=(f"exr{b}",))
            R.op("dve", lambda e, b=b: e.tensor_tensor(out=exr[b], in0=exr[b], in1=msk[b], op=ALU.mult), r=(f"exr{b}", f"msk{b}"), w=(f"exr{b}",))
            R.op("dve", lambda e, b=b: e.reduce_sum(out=mx8[b][:, 4:5], in_=exr[b], axis=AX.X), r=(f"exr{b}",), w=(f"mx8{b}",))
            R.op("dve", lambda e, b=b: e.reciprocal(out=mx8[b][:, 5:6], in_=mx8[b][:, 4:5]), r=(f"mx8{b}",), w=(f"mx8{b}",))
            R.op("dve", lambda e, b=b: e.tensor_scalar(out=cmb[b], in0=exr[b], scalar1=mx8[b][:, 5:6], scalar2=None, op0=ALU.mult),
                 r=(f"exr{b}", f"mx8{b}"), w=(f"cmb{b}",))
            R.op("pe", lambda e, b=b: e.transpose(out=ps[7][0:32, 0:128], in_=cmb[b], identity=ident[:]), r=(f"cmb{b}",), w=("ps7",))
            R.op("act", lambda e, i=i: e.activation(out=combT[:, i * 128:(i + 1) * 128], in_=ps[7][0:32, 0:128], func=AF.Copy),
                 r=("ps7",), w=(f"combT{i}",))
            if i == 2:
                dump("cmb2", cmb[b], [128, 32], (f"cmb{b}",))
                dump("lg2", lg[b], [128, 32], (f"lg{b}",))

        p5_stageA(0)
        for i in range(16):
            if i + 1 < 16:
                p5_stageA(i + 1)
            p5_stageB(i)
        dump("h2T", h2T[:, :, 256:384], [128, 8, 128], ("h2T2a", "h2T2b"), BF16)
        R.dma("sp", lambda e: e.dma_start(out=comb_d, in_=combT), r=tuple(f"combT{i}" for i in range(16)), w=("comb_d",), sem="d_comb")
        R.flush()
    if stop_after <= 5:
        return finish()

    top["A"] = 0
    with Mark(), Mark("A"):
        oacc = sb("oacc", [128, 8, 1024], ar="A")
        Dw = [sb(f"Dw{i}", [128, 8, 1024], BF16, ar="A") for i in range(2)]
        GUr = [sb(f"GUr{i}", [128, 8, 512], BF16) for i in range(3)]
        actT = sb("actT", [128, 8, 1024], BF16)
        cbc = [sb(f"cbc{i}", [128, 1024]) for i in range(2)]
        bguF = sb("bguF", [128, 512])
        bguS = sb("bguS", [128, 512])
        bdn = sb("bdn", [32, 1024])
        gp = [sb(f"gp{i}", [128, 512]) for i in range(2)]
        sg = [sb(f"sg{i}", [128, 512], BF16) for i in range(2)]
        ub = [sb(f"ub{i}", [128, 512]) for i in range(2)]
        uc = [sb(f"uc{i}", [128, 512]) for i in range(2)]
        t1 = [sb(f"t1_{i}", [128, 512], BF16) for i in range(2)]
        t2 = [sb(f"t2_{i}", [128, 512], BF16) for i in range(2)]
        fsv = sb("fsv", [128, 16, 8])
        fjunk = sb("fjunk", [128, 512], BF16)
        fin32 = actT.rearrange("p k f -> p (k f)").bitcast(F32)
        r1f = [fin32[:, 0:1024], fin32[:, 1024:2048]]
        outf = [fin32[:, 2048:3072], fin32[:, 3072:4096]]
        AK = ("actT0", "actT1")
        R.dma("sp", lambda e: e.dma_start(out=bguF, in_=bguF_d), w=("bguF",), sem="d_c")
        R.dma("sp", lambda e: e.dma_start(out=bdn, in_=b_down_d), w=("bdn",), sem="d_c")
        R.op("dve", lambda e: e.tensor_scalar(out=bguS, in0=bguF, scalar1=1.702, scalar2=None, op0=ALU.mult), r=("bguF",), w=("bguS",))

        NP = 2 * NE * 4

        def issue_piece(n):
            if n >= NP:
                return
            ex = (n // 4) % NE
            q4 = n % 4
            slot = n % 3
            wv = w_gu[ex].rearrange("(k p) f -> p k f", p=128)
            R.dma("pool", lambda e: e.dma_start(out=GUr[slot][:, :, 0:256], in_=wv[:, :, q4 * 256:(q4 + 1) * 256]),
                  w=(f"GUr{slot}",), sem=f"d_GU{slot}")
            R.dma("pool", lambda e: e.dma_start(out=GUr[slot][:, :, 256:512], in_=wv[:, :, 1024 + q4 * 256:1024 + (q4 + 1) * 256]),
                  w=(f"GUr{slot}",), sem=f"d_GU{slot}")

        def issue_dw(m):
            if m >= 2 * NE:
                return
            ex = m % NE
            wv = w_dn[ex].rearrange("(k p) f -> p k f", p=128)
            for q2 in range(2):
                R.dma("pool", lambda e, q2=q2: e.dma_start(out=Dw[m % 2][:, :, q2 * 512:(q2 + 1) * 512], in_=wv[:, :, q2 * 512:(q2 + 1) * 512]),
                      w=(f"Dw{m % 2}",), sem=f"d_Dw{m % 2}")

        def issue_cbc(m):
            if m >= 2 * NE:
                return
            ex = m % NE
            t0_ = (m // NE) * 1024
            R.dma("sp", lambda e: e.dma_start(out=cbc[m % 2], in_=comb_d[ex, t0_:t0_ + 1024].partition_broadcast(128)),
                  r=("comb_d",), w=(f"cbc{m % 2}",), sem=f"d_cbc{m % 2}")

        issue_piece(0)
        issue_piece(1)
        issue_dw(0)
        issue_cbc(0)
        it = 0
        dit = 0
        for half in range(2):
            t0 = half * 1024
            for tb in range(2):
                for dc in range(8):
                    pk = 4 + dc % 2
                    R.op("pe", lambda e, tb=tb, dc=dc, pk=pk, t0=t0: e.matmul(ps[pk][:, :], lhsT=bdn[:, dc * 128:(dc + 1) * 128],
                                                                              rhs=combT[:, t0 + tb * 512:t0 + (tb + 1) * 512], start=True, stop=True),
                         r=("bdn",), w=(f"ps{pk}",))
                    R.op("act", lambda e, tb=tb, dc=dc, pk=pk: e.activation(out=oacc[:, dc, tb * 512:(tb + 1) * 512], in_=ps[pk][:, :], func=AF.Copy),
                         r=(f"ps{pk}",), w=(f"oacc{dc}_{tb}",))
            for ex in range(NE):
                m = half * NE + ex
                issue_dw(m + 1)
                issue_cbc(m + 1)
                cb = m % 2
                for q4 in range(4):
                    n = m * 4 + q4
                    slot = n % 3
                    issue_piece(n + 2)
                    for tb in range(2):
                        tsl = slice(t0 + tb * 512, t0 + (tb + 1) * 512)
                        for fcl in range(2):
                            fc = q4 * 2 + fcl
                            p2 = it % 2
                            it += 1
                            pg, pu = 2 * p2, 2 * p2 + 1

                            def mmg(e, slot=slot, fcl=fcl, tsl=tsl, pg=pg, pu=pu):
                                ins = None
                                for k in range(8):
                                    ins = e.matmul(ps[pg][:, :], lhsT=GUr[slot][:, k, fcl * 128:(fcl + 1) * 128], rhs=h2T[:, k, tsl],
                                                   start=(k == 0), stop=(k == 7))
                                for k in range(8):
                                    ins = e.matmul(ps[pu][:, :], lhsT=GUr[slot][:, k, 256 + fcl * 128:256 + (fcl + 1) * 128], rhs=h2T[:, k, tsl],
                                                   start=(k == 0), stop=(k == 7))
                                return ins
                            R.op("pe", mmg, r=(f"GUr{slot}",), w=(f"ps{pg}", f"ps{pu}"))
                            bi = ex * 16 + fc
                            R.op("act", lambda e, p2=p2, pg=pg, bi=bi: e.activation(out=sg[p2], in_=ps[pg][:, :], func=AF.Sigmoid,
                                                                                   bias=bguS[:, bi:bi + 1], scale=1.702),
                                 r=(f"ps{pg}", "bguS"), w=(f"sg{p2}",))
                            R.op("act", lambda e, p2=p2, pu=pu, bi=bi: e.activation(out=ub[p2], in_=ps[pu][:, :], func=AF.Identity,
                                                                                   bias=bguF[:, bi + 8:bi + 9]),
                                 r=(f"ps{pu}", "bguF"), w=(f"ub{p2}",))
                            R.op("dve", lambda e, p2=p2, pg=pg, bi=bi: e.tensor_scalar(out=gp[p2], in0=ps[pg][:, :], scalar1=bguF[:, bi:bi + 1],
                                                                                      scalar2=7.0, op0=ALU.add, op1=ALU.min),
                                 r=(f"ps{pg}", "bguF", f"sg{p2}"), w=(f"gp{p2}",))
                            R.op("dve", lambda e, p2=p2: e.tensor_scalar(out=uc[p2], in0=ub[p2], scalar1=-7.0, scalar2=7.0, op0=ALU.max, op1=ALU.min),
                                 r=(f"ub{p2}",), w=(f"uc{p2}",))
                            R.op("dve", lambda e, p2=p2: e.tensor_tensor(out=t1[p2], in0=gp[p2], in1=sg[p2], op=ALU.mult),
                                 r=(f"gp{p2}", f"sg{p2}"), w=(f"t1_{p2}",))
                            R.op("dve", lambda e, p2=p2: e.scalar_tensor_tensor(out=t2[p2], in0=uc[p2], scalar=1.0, in1=t1[p2], op0=ALU.add, op1=ALU.mult),
                                 r=(f"uc{p2}", f"t1_{p2}"), w=(f"t2_{p2}",))
                            R.op("dve", lambda e, p2=p2, fc=fc, cb=cb, tb=tb: e.tensor_tensor(
                                out=actT[:, fc, tb * 512:(tb + 1) * 512], in0=t2[p2], in1=cbc[cb][:, tb * 512:(tb + 1) * 512], op=ALU.mult),
                                r=(f"t2_{p2}", f"cbc{cb}"), w=(f"actT{tb}",))
                for tb in range(2):
                    for dc in range(8):
                        pk = 4 + dit % 2
                        dit += 1

                        def mmd(e, m=m, dc=dc, pk=pk, tb=tb):
                            ins = None
                            for k in range(8):
                                ins = e.matmul(ps[pk][:, :], lhsT=Dw[m % 2][:, k, dc * 128:(dc + 1) * 128], rhs=actT[:, k, tb * 512:(tb + 1) * 512],
                                               start=(k == 0), stop=(k == 7))
                            return ins
                        R.op("pe", mmd, r=(f"Dw{m % 2}", f"actT{tb}"), w=(f"ps{pk}",))
                        R.op("dve", lambda e, dc=dc, tb=tb, pk=pk: e.tensor_tensor(out=oacc[:, dc, tb * 512:(tb + 1) * 512],
                                                                                  in0=oacc[:, dc, tb * 512:(tb + 1) * 512], in1=ps[pk][:, :], op=ALU.add),
                             r=(f"ps{pk}", f"oacc{dc}_{tb}"), w=(f"oacc{dc}_{tb}",))
            for ti in range(8):
                i = half * 8 + ti
                b = ti % 2
                R.dma("sp", lambda e, i=i, b=b: e.dma_start(out=r1f[b], in_=res1_d[i * 128:(i + 1) * 128, :]), r=(f"res1d{i}",),
                      w=(f"r1f{b}",) + AK, sem=f"d_r1f{b}")

                def trf(e, ti=ti):
                    ins = None
                    for dc in range(8):
                        ins = e.transpose(out=ps[6 + dc // 4][:, (dc % 4) * 128:(dc % 4 + 1) * 128], in_=oacc[:, dc, ti * 128:(ti + 1) * 128],
                                          identity=ident[:])
                    return ins
                R.op("pe", trf, r=tuple(f"oacc{dc}_{ti // 4}" for dc in range(8)), w=("ps6", "ps7"))
                for hf in range(2):
                    R.op("act", lambda e, i=i, hf=hf: e.activation(out=fjunk, in_=ps[6 + hf][:, :], func=AF.Square, accum_out=fsv[:, i, hf:hf + 1]),
                         r=(f"ps{6 + hf}",), w=("fjunk", f"fsv{i}_{hf}"))
                R.op("dve", lambda e, i=i: e.tensor_tensor(out=fsv[:, i, 2:3], in0=fsv[:, i, 0:1], in1=fsv[:, i, 1:2], op=ALU.add),
                     r=(f"fsv{i}_0", f"fsv{i}_1"), w=(f"fsv{i}_2",))
                R.op("dve", lambda e, i=i: e.tensor_scalar(out=fsv[:, i, 3:4], in0=fsv[:, i, 2:3], scalar1=1.0 / 1024, scalar2=EPS, op0=ALU.mult,
                                                           op1=ALU.add), r=(f"fsv{i}_2",), w=(f"fsv{i}_3",))
                R.op("act", lambda e, i=i: e.activation(out=fsv[:, i, 4:5], in_=fsv[:, i, 3:4], func=AF.Sqrt), r=(f"fsv{i}_3",), w=(f"fsv{i}_4",))
                R.op("dve", lambda e, i=i: e.reciprocal(out=fsv[:, i, 5:6], in_=fsv[:, i, 4:5]), r=(f"fsv{i}_4",), w=(f"fsv{i}_5",))
                for hf in range(2):
                    hsl = slice(hf * 512, (hf + 1) * 512)
                    R.op("dve", lambda e, i=i, b=b, hf=hf, hsl=hsl: e.scalar_tensor_tensor(out=outf[b][:, hsl], in0=ps[6 + hf][:, :], scalar=fsv[:, i, 5:6],
                                                                                          in1=Gf[:, hsl], op0=ALU.mult, op1=ALU.mult),
                         r=(f"ps{6 + hf}", f"fsv{i}_5"), w=(f"outf{b}_{hf}",) + AK)
                    R.op("dve", lambda e, b=b, hsl=hsl: e.tensor_tensor(out=outf[b][:, hsl], in0=outf[b][:, hsl], in1=r1f[b][:, hsl], op=ALU.add),
                         r=(f"outf{b}_{hf}", f"r1f{b}"), w=(f"outf{b}_{hf}",))
                R.dma("sp", lambda e, i=i, b=b: e.dma_start(out=out_d[i * 128:(i + 1) * 128, :], in_=outf[b]),
                      r=(f"outf{b}_0", f"outf{b}_1") + AK, w=(f"outd{i}",), sem="d_out")
        R.flush()
    return finish()


def _consts():
    idx = np.arange(128)
    rr, cc = idx[:, None], idx[None, :]
    masks = np.concatenate([(rr <= cc), (rr >= cc), (rr > cc), (rr < cc), np.ones((128, 128), bool)], axis=1).astype(np.float32)
    slopes = 2.0 ** (-8.0 * np.arange(1, 17) / 16.0)
    o = np.arange(17)
    delta = (o[None, :, None] - 8) * 128 + idx[:, None, None] - idx[None, None, :]
    ad = np.abs(delta)
    mult = (ad <= 64).astype(np.float64) + ((delta % 4 == 0) & (ad <= 256)) + ((delta % 16 == 0) & (ad <= 1024))
    etab = np.stack([mult * np.exp(-s * ad) for s in slopes]).astype(np.float32).reshape(16, 128, 17 * 128)
    sele = np.zeros((32, 32, 128), np.float32)
    for e in range(32):
        sele[e, e, :] = 1.0
    return masks, etab, sele.reshape(32, 32 * 128)


def make_in_maps(inp):
    f = lambda a: np.ascontiguousarray(np.asarray(a, dtype=np.float32))
    x = f(inp["x"])
    masks, etab, sele = _consts()
    common = {
        "w_ada": f(inp["w_ada"][0]), "b_ada_row": f(inp["b_ada"][0]),
        "g_pre_mix": f(inp["g_pre_mix"][0]), "g_pre_ffn": f(inp["g_pre_ffn"][0]),
        "g_post_mix": f(inp["g_post_mix"][0]), "g_post_ffn": f(inp["g_post_ffn"][0]), "g_ssd_norm": f(inp["g_ssd_norm"][0]),
        "w_in": f(inp["w_in"][0]),
        "convw": f(inp["conv_w"][0].reshape(5, 12, 128).transpose(2, 1, 0).reshape(128, 60)),
        "convb": f(inp["conv_b"][0].reshape(12, 128).T),
        "dt_bias": f(inp["dt_bias"][0].reshape(32)), "a_log": f(inp["a_log"][0].reshape(32)), "d_skip": f(inp["d_skip"][0]),
        "w_out": f(inp["w_out"][0]), "w_router": f(inp["w_router"][0]), "b_router": f(inp["b_router"][0]),
        "w_gate_up": f(inp["w_gate_up"][0]), "w_down": f(inp["w_down"][0]),
        "bguF": f(inp["b_gate_up"][0].reshape(32, 16, 128).transpose(2, 0, 1).reshape(128, 512)),
        "b_down": f(inp["b_down"][0]),
        "ident": np.eye(128, dtype=np.float32), "masks": masks, "etab": etab,
    }
    maps = []
    for r in range(NCORES):
        b, j = r // 4, r % 4
        lo = j * TOK - 1024
        xhp = np.zeros((4096, 1024), np.float32)
        s0, s1 = max(lo, 0), min(lo + 4096, 8192)
        xhp[s0 - lo:s1 - lo] = x[b, s0:s1]
        tokidx = lo + np.arange(4096)
        val = ((tokidx >= 0) & (tokidx < 8192)).astype(np.float32).reshape(32, 128).T
        edge = np.zeros((128, 2), np.float32)
        edge[:, 0] = 1.0 if j > 0 else 0.0
        edge[:, 1] = 1.0 if j < 3 else 0.0
        selv = np.zeros((128, 4), np.float32)
        selv[:, j] = 1.0
        m = dict(common)
        m.update({"xh": xhp, "valid": np.ascontiguousarray(val), "edge": edge, "sel": selv,
                  "cvec": f(np.asarray(inp["c"])[b].reshape(8, 128).T)})
        maps.append(m)
    return maps


def kernel(**inputs):
    nc, es, _ = build()
    with es:
        pass
    in_maps = make_in_maps(inputs)
    res = run_bass_kernel_spmd(nc, in_maps, core_ids=list(range(NCORES)))
    out = np.zeros((2, 8192, 1024), np.float32)
    for r in range(NCORES):
        b, j = r // 4, r % 4
        out[b, j * TOK:(j + 1) * TOK] = res.results[r]["out"]
    return out
```
